# Optimizing a Trainium2 kernel written in Bass

```python
import math
import jax
import jax.numpy as jnp
from jax import lax
import numpy as np

D_MODEL = 1024
BATCH = 32
SEQ = 2048
DEPTH = 4

N_MIXERS = 4
N_S5_LAYERS = (DEPTH + 3) // 4
N_MLSTM_LAYERS = (DEPTH + 2) // 4
N_FOX_LAYERS = (DEPTH + 1) // 4
N_RWKV_LAYERS = DEPTH // 4

NORM_EPS = 1e-6
D_FF = 4 * D_MODEL

S5_GROUP_CH = 16
S5_GROUPS = D_MODEL // S5_GROUP_CH
S5_STATE = 64
S5_DT_MIN = 1e-3
S5_DT_MAX = 1e-1

M_HEADS = 8
M_V_DIM = D_MODEL // M_HEADS
M_QK_DIM = M_V_DIM // 2
M_CONV = 4
M_CHUNK = 64
M_NORM_EPS = 1e-6
ML_IN = 2 * M_HEADS * M_QK_DIM + 2 * M_HEADS * M_V_DIM + 2 * M_HEADS

F_HEAD_DIM = 64
F_HEADS = D_MODEL // F_HEAD_DIM
F_QBLOCK = 128
F_IN = 4 * F_HEADS * F_HEAD_DIM + F_HEADS

R_HEAD_DIM = 64
R_HEADS = D_MODEL // R_HEAD_DIM
R_DECAY_LORA = int(max(32, round(1.8 * D_MODEL ** 0.5 / 32) * 32))
R_AAA_LORA = int(max(32, round(1.8 * D_MODEL ** 0.5 / 32) * 32))
R_GATE_LORA = int(max(32, round(0.6 * D_MODEL ** 0.8 / 32) * 32))
R_LN_EPS = 64e-5
R_IN = 3 * D_MODEL + R_DECAY_LORA + R_AAA_LORA + R_GATE_LORA

kernel_name = 'hybrid_s5_mlstm_fox_rwkv7_trunk'


def rms_norm(x, g):
    xf = x.astype(jnp.float32)
    y = xf * lax.rsqrt(jnp.mean(xf * xf, axis=-1, keepdims=True) + NORM_EPS)
    return (y * g.astype(jnp.float32)).astype(x.dtype)


def head_layer_norm(y, eps):
    y = y.astype(jnp.float32)
    yc = y - jnp.mean(y, axis=-1, keepdims=True)
    out = yc * lax.rsqrt(jnp.mean(yc * yc, axis=-1, keepdims=True) + eps)
    return out.reshape(out.shape[:-2] + (-1,))


def sq_relu_mlp(x, w_up, w_down):
    hid = jax.nn.relu(x @ w_up)
    return (hid * hid) @ w_down


def causal_depthwise_conv(x, w, b):
    width = w.shape[0]
    out = lax.conv_general_dilated(
        x, w[:, None, :].astype(x.dtype), window_strides=(1,), padding=[(width - 1, 0)],
        dimension_numbers=('NWC', 'WIO', 'NWC'), feature_group_count=x.shape[-1])
    return out + b


def _complex_linear_combine(left, right):
    a1r, a1i, b1r, b1i = left
    a2r, a2i, b2r, b2i = right
    return (a2r * a1r - a2i * a1i, a2r * a1i + a2i * a1r,
            a2r * b1r - a2i * b1i + b2r, a2r * b1i + a2i * b1r + b2i)


def s5_mixer(u, a_re, a_im, log_dt, b_re, b_im, c_re, c_im, d_skip, w_glu, b_glu):
    f32 = jnp.float32
    bsz, seq, d = u.shape
    dt = jnp.exp(log_dt.astype(f32))[:, None]
    ar, ai = a_re.astype(f32), a_im.astype(f32)
    mag = jnp.exp(dt * ar)
    abar_re, abar_im = mag * jnp.cos(dt * ai), mag * jnp.sin(dt * ai)
    inv = 1.0 / (ar * ar + ai * ai)
    zr, zi = abar_re - 1.0, abar_im
    coef_re = (zr * ar + zi * ai) * inv
    coef_im = (zi * ar - zr * ai) * inv
    br, bi = b_re.astype(f32), b_im.astype(f32)
    bbar_re = coef_re[..., None] * br - coef_im[..., None] * bi
    bbar_im = coef_re[..., None] * bi + coef_im[..., None] * br
    ug = u.astype(f32).reshape(bsz, seq, S5_GROUPS, S5_GROUP_CH)
    bu_re = jnp.einsum('blgh,gph->blgp', ug, bbar_re)
    bu_im = jnp.einsum('blgh,gph->blgp', ug, bbar_im)
    a_seq_re = jnp.broadcast_to(abar_re, (seq, S5_GROUPS, S5_STATE))
    a_seq_im = jnp.broadcast_to(abar_im, (seq, S5_GROUPS, S5_STATE))

    def scan_one(bre, bim):
        _, _, s_re, s_im = lax.associative_scan(
            _complex_linear_combine, (a_seq_re, a_seq_im, bre, bim), axis=0)
        return s_re, s_im

    s_re, s_im = jax.vmap(scan_one)(bu_re, bu_im)
    y = (jnp.einsum('blgp,ghp->blgh', s_re, c_re.astype(f32))
         - jnp.einsum('blgp,ghp->blgh', s_im, c_im.astype(f32)))
    y = y.reshape(bsz, seq, d) + d_skip.astype(f32) * u.astype(f32)
    z = jax.nn.gelu(y).astype(u.dtype) @ w_glu + b_glu
    out = z[..., :d] * jax.nn.sigmoid(z[..., d:])
    return out.astype(u.dtype)


def mlstm_mixer(x, w_in, conv_w, conv_b, b_i, b_f, head_g, w_out):
    f32 = jnp.float32
    bsz, seq, _ = x.shape
    nh, dk, dv, ch = M_HEADS, M_QK_DIM, M_V_DIM, M_CHUNK
    nc = seq // ch
    z = x @ w_in
    o0 = 2 * nh * dk
    o1 = o0 + nh * dv
    o2 = o1 + nh
    o3 = o2 + nh
    qk = jax.nn.silu(causal_depthwise_conv(z[..., :o0], conv_w, conv_b))
    q = qk[..., :nh * dk].reshape(bsz, seq, nh, dk) * (dk ** -0.5)
    k = qk[..., nh * dk:].reshape(bsz, seq, nh, dk)
    v = z[..., o0:o1].reshape(bsz, seq, nh, dv)
    log_i = (z[..., o1:o2] + b_i).astype(f32)
    log_f = jax.nn.log_sigmoid((z[..., o2:o3] + b_f).astype(f32))
    o_gate = jax.nn.sigmoid(z[..., o3:])

    def chunks4(t):
        return t.astype(f32).reshape(bsz, nc, ch, nh, -1).transpose(1, 0, 3, 2, 4)

    def chunks3(t):
        return t.reshape(bsz, nc, ch, nh).transpose(1, 0, 3, 2)

    causal = jnp.tril(jnp.ones((ch, ch), dtype=bool))

    def step(carry, inp):
        c_mat, n_vec, m_prev = carry
        qc, kc, vc, lic, lfc = inp
        b = jnp.cumsum(lfc, axis=-1)
        g = b + m_prev[..., None]
        d_log = jnp.where(causal, b[..., :, None] - b[..., None, :] + lic[..., None, :], -jnp.inf)
        m_t = jnp.maximum(g, jnp.max(d_log, axis=-1))
        w_inter = jnp.exp(g - m_t)
        s = jnp.einsum('bhtd,bhsd->bhts', qc, kc) * jnp.exp(d_log - m_t[..., None])
        num = (w_inter[..., None] * jnp.einsum('bhtd,bhde->bhte', qc, c_mat)
               + jnp.einsum('bhts,bhse->bhte', s, vc))
        den = w_inter * jnp.einsum('bhtd,bhd->bht', qc, n_vec) + jnp.sum(s, axis=-1)
        h = num / jnp.maximum(jnp.abs(den), jnp.exp(-m_t))[..., None]
        m_new = m_t[..., -1]
        w_state = jnp.exp(b[..., -1:] - b + lic - m_new[..., None])
        decay = jnp.exp(b[..., -1] + m_prev - m_new)
        c_new = decay[..., None, None] * c_mat + jnp.einsum('bhsd,bhse->bhde', kc * w_state[..., None], vc)
        n_new = decay[..., None] * n_vec + jnp.einsum('bhs,bhsd->bhd', w_state, kc)
        return (c_new, n_new, m_new), h

    init = (jnp.zeros((bsz, nh, dk, dv), f32), jnp.zeros((bsz, nh, dk), f32), jnp.zeros((bsz, nh), f32))
    _, h = lax.scan(step, init, (chunks4(q), chunks4(k), chunks4(v), chunks3(log_i), chunks3(log_f)))
    h = h.transpose(1, 0, 3, 2, 4).reshape(bsz, seq, nh, dv)
    h = head_layer_norm(h, M_NORM_EPS) * head_g.astype(f32)
    out = (h * o_gate.astype(f32)).astype(x.dtype) @ w_out
    return out.astype(x.dtype)


def fox_mixer(x, w_in, b_f, w_out):
    f32 = jnp.float32
    bsz, seq, _ = x.shape
    nh, dh, qb = F_HEADS, F_HEAD_DIM, F_QBLOCK
    hd = nh * dh
    z = x @ w_in

    def heads(t):
        return t.reshape(bsz, seq, nh, dh).transpose(0, 2, 1, 3)

    q, k, v = heads(z[..., :hd]), heads(z[..., hd:2 * hd]), heads(z[..., 2 * hd:3 * hd])
    log_f = jax.nn.log_sigmoid((z[..., 3 * hd:3 * hd + nh] + b_f).astype(f32))
    o_gate = jax.nn.sigmoid(z[..., 3 * hd + nh:])
    cum = jnp.cumsum(log_f, axis=1).transpose(0, 2, 1)
    scale = dh ** -0.5
    outs = []
    for blk in range(seq // qb):
        t0, t1 = blk * qb, (blk + 1) * qb
        s = jnp.einsum('bhtd,bhsd->bhts', q[:, :, t0:t1], k[:, :, :t1]).astype(f32) * scale
        s = s + (cum[:, :, t0:t1, None] - cum[:, :, None, :t1])
        mask = (t0 + jnp.arange(qb))[:, None] >= jnp.arange(t1)[None, :]
        p = jax.nn.softmax(jnp.where(mask, s, -jnp.inf), axis=-1)
        outs.append(jnp.einsum('bhts,bhsd->bhtd', p.astype(v.dtype), v[:, :, :t1]))
    o = jnp.concatenate(outs, axis=2).transpose(0, 2, 1, 3).reshape(bsz, seq, hd)
    return ((o * o_gate) @ w_out).astype(x.dtype)


def rwkv7_mixer(x, mu, w_in, w0, w_up, a0, a_up, g_up, k_k, k_a, r_k, ln_g, ln_b, w_out):
    f32 = jnp.float32
    bsz, seq, d = x.shape
    nh, dh = R_HEADS, R_HEAD_DIM
    x_prev = jnp.pad(x, ((0, 0), (1, 0), (0, 0)))[:, :-1]
    dx = x_prev - x
    xr, xw, xk, xv, xa, xg = (x + dx * mu[idx] for idx in range(6))
    c0 = 3 * d
    c1 = c0 + R_DECAY_LORA
    c2 = c1 + R_AAA_LORA
    r = xr @ w_in[:, :d]
    k = xk @ w_in[:, d:2 * d]
    v = xv @ w_in[:, 2 * d:c0]
    w_log = -jax.nn.softplus(-(w0 + jnp.tanh(xw @ w_in[:, c0:c1]) @ w_up).astype(f32)) - 0.5
    decay = jnp.exp(-jnp.exp(w_log))
    a = jax.nn.sigmoid((a0 + (xa @ w_in[:, c1:c2]) @ a_up).astype(f32))
    g = jax.nn.sigmoid(xg @ w_in[:, c2:]) @ g_up

    def heads(t):
        return t.astype(f32).reshape(bsz, seq, nh, dh)

    kk = heads(k * k_k)
    kk = kk / jnp.maximum(jnp.sqrt(jnp.sum(kk * kk, axis=-1, keepdims=True)), 1e-12)
    k_mod = k.astype(f32) * (1.0 + (a - 1.0) * k_a.astype(f32))
    rh, wh, kh, vh, ah = heads(r), heads(decay), heads(k_mod), heads(v), heads(a)

    def time_major(t):
        return t.transpose(1, 0, 2, 3)

    def step(state, inp):
        r_t, w_t, k_t, v_t, kk_t, a_t = inp
        sa = jnp.einsum('bhij,bhj->bhi', state, -kk_t)
        state = (state * w_t[:, :, None, :] + sa[..., None] * (kk_t * a_t)[:, :, None, :]
                 + v_t[..., None] * k_t[:, :, None, :])
        return state, jnp.einsum('bhij,bhj->bhi', state, r_t)

    s0 = jnp.zeros((bsz, nh, dh, dh), f32)
    _, y = lax.scan(step, s0, (time_major(rh), time_major(wh), time_major(kh),
                               time_major(vh), time_major(kk), time_major(ah)))
    y = time_major(y)
    bonus = jnp.sum(rh * kh * r_k.astype(f32), axis=-1, keepdims=True) * vh
    y = (head_layer_norm(y, R_LN_EPS) * ln_g.astype(f32) + ln_b.astype(f32)
         + bonus.reshape(bsz, seq, d)) * g.astype(f32)
    return (y.astype(x.dtype) @ w_out).astype(x.dtype)


def setup_inputs(seed: int = 0) -> dict:
    key = jax.random.key(seed)
    ks = iter(jax.random.split(key, 64))
    f32 = jnp.float32
    d = D_MODEL
    n_a, n_b, n_c, n_d = N_S5_LAYERS, N_MLSTM_LAYERS, N_FOX_LAYERS, N_RWKV_LAYERS

    def nrm(shape, std):
        return std * jax.random.normal(next(ks), shape, f32)

    def gain(shape):
        return 1.0 + nrm(shape, 0.02)

    inp = {}
    inp['x'] = jax.random.normal(next(ks), (BATCH, SEQ, d), f32)
    inp['mlp_norm_g'] = gain((DEPTH, d))
    inp['mlp_w_up'] = nrm((DEPTH, d, D_FF), d ** -0.5)
    inp['mlp_w_down'] = nrm((DEPTH, D_FF, d), D_FF ** -0.5)
    inp['final_norm_g'] = gain((d,))
    n_idx = jnp.arange(S5_STATE, dtype=f32)
    inp['s5_norm_g'] = gain((n_a, d))
    inp['s5_a_re'] = -0.5 + nrm((n_a, S5_GROUPS, S5_STATE), 0.01)
    inp['s5_a_im'] = jnp.pi * n_idx + nrm((n_a, S5_GROUPS, S5_STATE), 0.01)
    inp['s5_log_dt'] = jax.random.uniform(next(ks), (n_a, S5_GROUPS), f32,
                                          math.log(S5_DT_MIN), math.log(S5_DT_MAX))
    inp['s5_b_re'] = nrm((n_a, S5_GROUPS, S5_STATE, S5_GROUP_CH), (2 * S5_GROUP_CH) ** -0.5)
    inp['s5_b_im'] = nrm((n_a, S5_GROUPS, S5_STATE, S5_GROUP_CH), (2 * S5_GROUP_CH) ** -0.5)
    inp['s5_c_re'] = nrm((n_a, S5_GROUPS, S5_GROUP_CH, S5_STATE), S5_STATE ** -0.5)
    inp['s5_c_im'] = nrm((n_a, S5_GROUPS, S5_GROUP_CH, S5_STATE), S5_STATE ** -0.5)
    inp['s5_d'] = nrm((n_a, d), 1.0)
    inp['s5_w_glu'] = nrm((n_a, d, 2 * d), d ** -0.5)
    inp['s5_b_glu'] = nrm((n_a, 2 * d), 0.01)
    inp['ml_norm_g'] = gain((n_b, d))
    inp['ml_w_in'] = nrm((n_b, d, ML_IN), d ** -0.5)
    inp['ml_conv_w'] = nrm((n_b, M_CONV, 2 * M_HEADS * M_QK_DIM), M_CONV ** -0.5)
    inp['ml_conv_b'] = nrm((n_b, 2 * M_HEADS * M_QK_DIM), 0.01)
    inp['ml_b_i'] = nrm((n_b, M_HEADS), 0.1)
    inp['ml_b_f'] = jnp.linspace(3.0, 6.0, M_HEADS, dtype=f32) + nrm((n_b, M_HEADS), 0.01)
    inp['ml_head_g'] = gain((n_b, M_HEADS * M_V_DIM))
    inp['ml_w_out'] = nrm((n_b, M_HEADS * M_V_DIM, d), (M_HEADS * M_V_DIM) ** -0.5)
    inp['fox_norm_g'] = gain((n_c, d))
    inp['fox_w_in'] = nrm((n_c, d, F_IN), d ** -0.5)
    inp['fox_b_f'] = jnp.linspace(1.0, 6.0, F_HEADS, dtype=f32) + nrm((n_c, F_HEADS), 0.01)
    inp['fox_w_out'] = nrm((n_c, F_HEADS * F_HEAD_DIM, d), (F_HEADS * F_HEAD_DIM) ** -0.5)
    inp['rw_norm_g'] = gain((n_d, d))
    inp['rw_mu'] = jax.random.uniform(next(ks), (n_d, 6, d), f32)
    inp['rw_w_in'] = nrm((n_d, d, R_IN), d ** -0.5)
    inp['rw_w0'] = jnp.linspace(-6.5, -1.5, d, dtype=f32) + nrm((n_d, d), 0.01)
    inp['rw_w_up'] = nrm((n_d, R_DECAY_LORA, d), 0.1 * R_DECAY_LORA ** -0.5)
    inp['rw_a0'] = nrm((n_d, d), 0.1)
    inp['rw_a_up'] = nrm((n_d, R_AAA_LORA, d), R_AAA_LORA ** -0.5)
    inp['rw_g_up'] = nrm((n_d, R_GATE_LORA, d), R_GATE_LORA ** -0.5)
    inp['rw_k_k'] = 0.85 + nrm((n_d, d), 0.02)
    inp['rw_k_a'] = 1.0 + nrm((n_d, d), 0.02)
    inp['rw_r_k'] = -0.04 + nrm((n_d, R_HEADS, R_HEAD_DIM), 0.02)
    inp['rw_ln_g'] = gain((n_d, d))
    inp['rw_ln_b'] = nrm((n_d, d), 0.01)
    inp['rw_w_out'] = nrm((n_d, d, d), d ** -0.5)
    return inp


def reference(x, mlp_norm_g, mlp_w_up, mlp_w_down, final_norm_g,
              s5_norm_g, s5_a_re, s5_a_im, s5_log_dt, s5_b_re, s5_b_im, s5_c_re, s5_c_im,
              s5_d, s5_w_glu, s5_b_glu,
              ml_norm_g, ml_w_in, ml_conv_w, ml_conv_b, ml_b_i, ml_b_f, ml_head_g, ml_w_out,
              fox_norm_g, fox_w_in, fox_b_f, fox_w_out,
              rw_norm_g, rw_mu, rw_w_in, rw_w0, rw_w_up, rw_a0, rw_a_up, rw_g_up,
              rw_k_k, rw_k_a, rw_r_k, rw_ln_g, rw_ln_b, rw_w_out):
    h = x
    for i in range(DEPTH):
        kind, j = i % N_MIXERS, i // N_MIXERS
        if kind == 0:
            mix = s5_mixer(rms_norm(h, s5_norm_g[j]), s5_a_re[j], s5_a_im[j], s5_log_dt[j],
                           s5_b_re[j], s5_b_im[j], s5_c_re[j], s5_c_im[j], s5_d[j],
                           s5_w_glu[j], s5_b_glu[j])
        elif kind == 1:
            mix = mlstm_mixer(rms_norm(h, ml_norm_g[j]), ml_w_in[j], ml_conv_w[j], ml_conv_b[j],
                              ml_b_i[j], ml_b_f[j], ml_head_g[j], ml_w_out[j])
        elif kind == 2:
            mix = fox_mixer(rms_norm(h, fox_norm_g[j]), fox_w_in[j], fox_b_f[j], fox_w_out[j])
        else:
            mix = rwkv7_mixer(rms_norm(h, rw_norm_g[j]), rw_mu[j], rw_w_in[j], rw_w0[j], rw_w_up[j],
                              rw_a0[j], rw_a_up[j], rw_g_up[j], rw_k_k[j], rw_k_a[j], rw_r_k[j],
                              rw_ln_g[j], rw_ln_b[j], rw_w_out[j])
        h = h + mix
        h = h + sq_relu_mlp(rms_norm(h, mlp_norm_g[i]), mlp_w_up[i], mlp_w_down[i]).astype(h.dtype)
    return rms_norm(h, final_norm_g)
```

```python
import numpy as np
import concourse.bass as bass
import concourse.mybir as mybir
from concourse.bass_utils import run_bass_kernel_spmd

F32 = mybir.dt.float32
BF16 = mybir.dt.bfloat16
AF = mybir.ActivationFunctionType
ALU = mybir.AluOpType
AX = mybir.AxisListType

COMPUTE = ("tensor", "vector", "scalar", "gpsimd")
ENGS = ("sync",) + COMPUTE

D = 1024
DFF = 4096
EPS = 1e-6


class Prog:
    def __init__(self, nc):
        self.nc = nc
        self.ops = []
        self.ctx = []
        self.ntiles = 0
        self.pending = {}
        self.last_eng = {}
        self.last_dma = {}
        self.marks = []

    def sb(self, shape, dt, name=None):
        self.ntiles += 1
        cm = self.nc.sbuf_tensor(f"{name or 't'}_{self.ntiles}", list(shape), dt)
        t = cm.__enter__()
        self.ctx.append(cm)
        return t

    def ps(self, shape, dt, name=None):
        self.ntiles += 1
        cm = self.nc.psum_tensor(f"{name or 'p'}_{self.ntiles}", list(shape), dt)
        t = cm.__enter__()
        self.ctx.append(cm)
        return t

    def push(self):
        self.marks.append(len(self.ctx))

    def pop(self):
        m = self.marks.pop()
        while len(self.ctx) > m:
            self.ctx.pop().__exit__(None, None, None)
        self.barrier()

    def barrier(self):
        deps = list(self.last_eng.values()) + list(self.last_dma.values())
        for e in ENGS:
            self.pending[e] = deps

    def _rec(self, o):
        o["extra"] = self.pending.pop(o["eng"], ())
        i = len(self.ops)
        self.ops.append(o)
        if o["dma"]:
            self.last_dma[o["semkey"]] = i
        else:
            self.last_eng[o["eng"]] = i

    def op(self, eng, fn, reads=(), writes=()):
        self._rec(dict(eng=eng, fn=fn, reads=tuple(reads), writes=tuple(writes), dma=False, semkey=None))

    def dma(self, eng, out, in_, reads=(), writes=(), semkey=None, **kw):
        assert semkey is not None
        self._rec(dict(eng=eng, fn=lambda e: e.dma_start(out=out, in_=in_, **kw), reads=tuple(reads),
                       writes=tuple(writes), dma=True, semkey=semkey))

    def finish(self, keys):
        self._rec(dict(eng="sync", fn=None, reads=tuple(keys), writes=(), dma=False, semkey=None))

    def emit(self):
        nc = self.nc
        ops = self.ops
        last_w = {}
        readers = {}
        per_eng_idx = {e: 0 for e in ENGS}
        dma_cnt = {}
        for i, o in enumerate(ops):
            if o["dma"]:
                dma_cnt[o["semkey"]] = dma_cnt.get(o["semkey"], 0) + 16
                o["ticket"] = ("d", o["semkey"], dma_cnt[o["semkey"]])
            else:
                per_eng_idx[o["eng"]] += 1
                o["ticket"] = ("c", o["eng"], per_eng_idx[o["eng"]])
        known = {e: {} for e in ENGS}
        cur_cnt = {}
        snap = {}
        signaling = set()
        for i, o in enumerate(ops):
            deps = set(o["extra"])
            for k in o["reads"]:
                if k in last_w:
                    deps.add(last_w[k])
            for k in o["writes"]:
                if k in last_w:
                    deps.add(last_w[k])
                deps.update(readers.get(k, ()))
            for k in o["reads"]:
                readers.setdefault(k, []).append(i)
            for k in o["writes"]:
                last_w[k] = i
                readers[k] = []
            X = o["eng"]
            need = {}
            if o["dma"]:
                cur_cnt[o["semkey"]] = o["ticket"][2]
            for d in deps:
                if d == i:
                    continue
                t = ops[d]["ticket"]
                if t[0] == "d":
                    t = (t[0], t[1], cur_cnt[t[1]] - (16 if (o["dma"] and o["semkey"] == t[1]) else 0))
                if t[0] == "c" and t[1] == "tensor" and X == "tensor" and not o["dma"]:
                    continue
                key = (t[0], t[1])
                if known[X].get(key, 0) >= t[2]:
                    continue
                if need.get(key, (0, None))[0] < t[2]:
                    need[key] = (t[2], d)
            waits = []
            for key, (val, d) in need.items():
                waits.append((key, val, d))
                known[X][key] = max(known[X].get(key, 0), val)
                if key[0] == "c":
                    signaling.add(d)
                    for kk, vv in snap[d].items():
                        if known[X].get(kk, 0) < vv:
                            known[X][kk] = vv
            o["waits"] = waits
            if not o["dma"]:
                snap[i] = dict(known[X])
        sigcount = {e: 0 for e in ENGS}
        for i, o in enumerate(ops):
            if not o["dma"] and i in signaling:
                sigcount[o["eng"]] += 1
                o["sig"] = sigcount[o["eng"]]
            else:
                o["sig"] = None
        sems = {}
        for e in ENGS:
            if sigcount[e]:
                cm = nc.semaphore(f"s_{e}")
                sems[("c", e)] = cm.__enter__()
                self.ctx.append(cm)
        for j, k in enumerate(sorted(dma_cnt, key=str)):
            cm = nc.semaphore(f"d{j}")
            sems[("d", k)] = cm.__enter__()
            self.ctx.append(cm)
        self.stats = dict(n_ops=len(ops), per_eng=dict(per_eng_idx), sig=dict(sigcount), nsems=len(sems),
                          nwaits=sum(len(o["waits"]) for o in ops))
        by_eng = {e: [o for o in ops if o["eng"] == e] for e in ENGS}

        def run(e, lst):
            for o in lst:
                for key, val, d in o["waits"]:
                    if key[0] == "c":
                        e.wait_ge(sems[key], ops[d]["sig"])
                    else:
                        e.wait_ge(sems[key], val)
                if o["fn"] is None:
                    continue
                ins = o["fn"](e)
                if o["dma"]:
                    ins.then_inc(sems[("d", o["semkey"])], 16)
                elif o["sig"] is not None:
                    ins.then_inc(sems[("c", o["eng"])], 1)

        with nc.Block() as block:
            @block.sync
            def _(e):
                run(e, by_eng["sync"])

            @block.tensor
            def _(e):
                run(e, by_eng["tensor"])

            @block.vector
            def _(e):
                run(e, by_eng["vector"])

            @block.scalar
            def _(e):
                run(e, by_eng["scalar"])

            @block.gpsimd
            def _(e):
                run(e, by_eng["gpsimd"])

    def close(self):
        for cm in reversed(self.ctx):
            cm.__exit__(None, None, None)
        self.ctx = []


class Rot:
    def __init__(self, items):
        self.items = items
        self.i = 0

    def next(self):
        it = self.items[self.i % len(self.items)]
        self.i += 1
        return it


def hkeys(tile, cols=None):
    if cols is None:
        return [("h", tile, c) for c in range(4)]
    return [("h", tile, c) for c in cols]


class Ctx:
    def __init__(self, nc, P):
        self.nc = nc
        self.P = P
        self.identf = P.sb([128, 128], F32, "identf")
        self.ident = P.sb([128, 128], BF16, "ident")
        P.op("gpsimd", lambda e: e.memset(self.identf[:], 0.0), writes=["identf"])
        P.op("gpsimd", lambda e: e.affine_select(out=self.identf[:], in_=self.identf[:], pattern=[[-1, 128]],
                                                 compare_op=ALU.not_equal, fill=1.0, base=0, channel_multiplier=1),
             reads=["identf"], writes=["identf"])
        P.op("vector", lambda e: e.tensor_copy(out=self.ident[:], in_=self.identf[:]), reads=["identf"], writes=["ident"])
        self.ht = Rot([(P.sb([128, D], F32, f"ht{i}"), f"ht{i}") for i in range(2)])
        self.gb = P.sb([128, D], F32, "gb")
        self.sq = P.sb([128, D], F32, "sq")
        self.ss = Rot([(P.sb([128, 1], F32, f"ss{i}"), f"ss{i}") for i in range(2)])
        self.xn = Rot([(P.sb([128, D], BF16, f"xn{i}"), f"xn{i}") for i in range(2)])
        self.pT = Rot([(P.ps([128, 8, 128], BF16, f"pT{i}"), f"pT{i}") for i in range(2)])
        self.pA = Rot([(P.ps([128, 512], F32, f"pA{i}"), f"pA{i}") for i in range(4)])
        self.cp = 0
        self.onesf = P.sb([128, 128], F32, "onesf")
        self.tri = P.sb([128, 128], F32, "tri")
        self.mask01 = P.sb([128, 128], BF16, "mask01")
        P.op("gpsimd", lambda e: e.memset(self.onesf[:], 1.0), writes=["onesf"])
        P.op("gpsimd", lambda e: e.affine_select(out=self.tri[:], in_=self.onesf[:], pattern=[[1, 128]],
                                                 compare_op=ALU.is_ge, fill=0.0, base=0, channel_multiplier=-1),
             reads=["onesf"], writes=["tri"])
        P.op("vector", lambda e: e.tensor_copy(out=self.mask01[:], in_=self.tri[:]), reads=["tri"], writes=["mask01"])

    def copy_eng(self):
        self.cp += 1
        return "vector" if self.cp % 2 else "scalar"

    def evac(self, out, in_, reads, writes, eng=None):
        eng = eng or self.copy_eng()
        if eng == "vector":
            self.P.op("vector", lambda e: e.tensor_copy(out=out, in_=in_), reads=reads, writes=writes)
        else:
            self.P.op("scalar", lambda e: e.copy(out=out, in_=in_), reads=reads, writes=writes)


def load_gain(C, g_ap):
    C.P.dma("sync", C.gb[:], g_ap.broadcast_to([128, D]), writes=["gb"], semkey="gb")


def norm_tile(C, hsrc, tile, xn_out=None):
    P = C.P
    ht, hk = C.ht.next()
    ss, sk = C.ss.next()
    xn, xk = C.xn.next()
    P.dma("sync", ht[:], hsrc[tile * 128:(tile + 1) * 128, :], reads=hkeys(tile), writes=[hk], semkey=hk)
    P.op("scalar", lambda e: e.activation(out=C.sq[:], in_=ht[:], func=AF.Square, accum_out=ss[:]),
         reads=[hk], writes=["sq", sk])
    P.op("scalar", lambda e: e.activation(out=ss[:], in_=ss[:], func=AF.Sqrt, scale=1.0 / D, bias=EPS),
         reads=[sk], writes=[sk])
    P.op("vector", lambda e: e.reciprocal(out=ss[:], in_=ss[:]), reads=[sk], writes=[sk])
    P.op("vector", lambda e: e.scalar_tensor_tensor(out=xn[:], in0=ht[:], scalar=ss[:, 0:1], in1=C.gb[:],
                                                    op0=ALU.mult, op1=ALU.mult),
         reads=[hk, sk, "gb"], writes=[xk])
    return xn, xk


def norm_T(C, hsrc, tile0, ntiles, xnT, xnT_key):
    P = C.P
    for n in range(ntiles):
        xn, xk = norm_tile(C, hsrc, tile0 + n)
        pT, pk = C.pT.next()
        for c in range(8):
            P.op("tensor", lambda e, c=c, pT=pT, xn=xn: e.transpose(out=pT[:, c, :], in_=xn[:, c * 128:(c + 1) * 128],
                                                                  identity=C.ident[:]),
                 reads=[xk, "ident"], writes=[pk])
        C.evac(xnT[:, :, n * 128:(n + 1) * 128], pT[:], [pk], [(xnT_key, n)])


def stage_mlp(C, hsrc, hdst, tile0, w_up, w_down, g_ap):
    P = C.P
    P.push()
    NTB = 8
    TB = NTB * 128
    xnT = P.sb([128, 8, TB], BF16, "m_xnT")
    hid = P.sb([128, 32, TB], BF16, "m_hid")
    wup = Rot([(P.sb([128, 8, 512], BF16, f"m_wup{i}"), f"m_wup{i}") for i in range(2)])
    wdn = Rot([(P.sb([128, 32, 256], BF16, f"m_wdn{i}"), f"m_wdn{i}") for i in range(2)])
    relu = Rot([(P.sb([128, 512], F32, f"m_relu{i}"), f"m_relu{i}") for i in range(2)])
    hsr = Rot([(P.sb([128, 256], F32, f"m_hs{i}"), f"m_hs{i}") for i in range(3)])
    load_gain(C, g_ap)
    norm_T(C, hsrc, tile0, NTB, xnT, "xnT")
    wup_v = w_up.rearrange("(c p) n -> p c n", p=128)
    wdn_v = w_down.rearrange("(c p) n -> p c n", p=128)
    for jb in range(8):
        wt, wk = wup.next()
        P.dma("gpsimd", wt[:], wup_v[:, :, jb * 512:(jb + 1) * 512], writes=[wk], semkey=wk)
        for j4 in range(4):
            j = jb * 4 + j4
            for tb in range(TB // 512):
                ps, pk = C.pA.next()
                for c in range(8):
                    P.op("tensor", lambda e, c=c, ps=ps, wt=wt, j4=j4, tb=tb: e.matmul(
                        out=ps[:], lhsT=wt[:, c, j4 * 128:(j4 + 1) * 128], rhs=xnT[:, c, tb * 512:(tb + 1) * 512],
                        start=(c == 0), stop=(c == 7)),
                        reads=[wk] + [("xnT", tb * 4 + q) for q in range(4)], writes=[pk])
                rt, rk = relu.next()
                P.op("scalar", lambda e, rt=rt, ps=ps: e.activation(out=rt[:], in_=ps[:], func=AF.Relu), reads=[pk], writes=[rk])
                P.op("vector", lambda e, rt=rt, ps=ps, j=j, tb=tb: e.tensor_tensor(
                    out=hid[:, j, tb * 512:(tb + 1) * 512], in0=rt[:], in1=ps[:], op=ALU.mult),
                    reads=[rk, pk], writes=[("hid", j, tb)])
    for nb in range(4):
        wt, wk = wdn.next()
        P.dma("gpsimd", wt[:], wdn_v[:, :, nb * 256:(nb + 1) * 256], writes=[wk], semkey=wk)
        for tt in range(NTB):
            ps, pk = C.pA.next()
            for j in range(32):
                P.op("tensor", lambda e, j=j, ps=ps, wt=wt, tt=tt: e.matmul(
                    out=ps[:, 0:256], lhsT=hid[:, j, tt * 128:(tt + 1) * 128], rhs=wt[:, j, :],
                    start=(j == 0), stop=(j == 31)),
                    reads=[wk, ("hid", j, tt // 4)], writes=[pk])
            hs, hk = hsr.next()
            tile = tile0 + tt
            P.dma("sync", hs[:], hsrc[tile * 128:(tile + 1) * 128, nb * 256:(nb + 1) * 256],
                  reads=hkeys(tile, [nb]), writes=[hk], semkey=hk)
            P.op("vector", lambda e, hs=hs, ps=ps: e.tensor_tensor(out=hs[:], in0=hs[:], in1=ps[:, 0:256], op=ALU.add),
                 reads=[hk, pk], writes=[hk])
            P.dma("sync", hdst[tile * 128:(tile + 1) * 128, nb * 256:(nb + 1) * 256], hs[:],
                  reads=[hk], writes=hkeys(tile, [nb]), semkey=hk)
    P.pop()


def stage_final(C, hsrc, y, ntiles, g_ap):
    P = C.P
    load_gain(C, g_ap)
    for n in range(ntiles):
        ht, hk = C.ht.next()
        ss, sk = C.ss.next()
        P.dma("sync", ht[:], hsrc[n * 128:(n + 1) * 128, :], reads=hkeys(n), writes=[hk], semkey=hk)
        P.op("scalar", lambda e, ht=ht, ss=ss: e.activation(out=C.sq[:], in_=ht[:], func=AF.Square, accum_out=ss[:]),
             reads=[hk], writes=["sq", sk])
        P.op("scalar", lambda e, ss=ss: e.activation(out=ss[:], in_=ss[:], func=AF.Sqrt, scale=1.0 / D, bias=EPS),
             reads=[sk], writes=[sk])
        P.op("vector", lambda e, ss=ss: e.reciprocal(out=ss[:], in_=ss[:]), reads=[sk], writes=[sk])
        P.op("vector", lambda e, ht=ht, ss=ss: e.scalar_tensor_tensor(out=ht[:], in0=ht[:], scalar=ss[:, 0:1], in1=C.gb[:],
                                                                  op0=ALU.mult, op1=ALU.mult),
             reads=[hk, sk, "gb"], writes=[hk])
        P.dma("sync", y[n * 128:(n + 1) * 128, :], ht[:], reads=[hk], writes=[("y", n)], semkey=hk)


def gate_cumsum(C, lf, NT, NH, pfx):
    P = C.P
    cum = P.sb([128, NT, NH], F32, pfx + "cum")
    tot = P.sb([128, NT, NH], F32, pfx + "tot")
    carry = P.sb([128, NT + 1, NH], F32, pfx + "carry")
    ps1, k1 = C.pA.next()
    ps2, k2 = C.pA.next()
    lf2 = lf[:].rearrange("p n h -> p (n h)")
    P.op("tensor", lambda e: e.matmul(out=ps1[:, 0:NT * NH], lhsT=C.tri[:], rhs=lf2, start=True, stop=True),
         reads=[pfx + "lf", "tri"], writes=[k1])
    P.op("tensor", lambda e: e.matmul(out=ps2[:, 0:NT * NH], lhsT=C.onesf[:], rhs=lf2, start=True, stop=True),
         reads=[pfx + "lf", "onesf"], writes=[k2])
    P.op("vector", lambda e: e.tensor_copy(out=tot[:].rearrange("p n h -> p (n h)"), in_=ps2[:, 0:NT * NH]),
         reads=[k2], writes=[pfx + "tot"])
    P.op("vector", lambda e: e.memset(carry[:, 0, :], 0.0), writes=[pfx + "carry"])
    for n in range(NT):
        P.op("vector", lambda e, n=n: e.tensor_tensor(out=carry[:, n + 1, :], in0=carry[:, n, :], in1=tot[:, n, :], op=ALU.add),
             reads=[pfx + "carry", pfx + "tot"], writes=[pfx + "carry"])
    P.op("vector", lambda e: e.tensor_tensor(out=cum[:].rearrange("p n h -> p (n h)"), in0=ps1[:, 0:NT * NH],
                                             in1=carry[:, 0:NT, :].rearrange("p n h -> p (n h)"), op=ALU.add),
         reads=[k1, pfx + "carry"], writes=[pfx + "cum"])
    return cum, carry, tot


def log_sigmoid_tok(C, ps, pk, bias_bc, bias_key, out, out_key, NT, NH, tmp, tmp_key):
    P = C.P
    P.op("vector", lambda e: e.tensor_tensor(out=tmp[:], in0=ps[:, 0:NT * NH].rearrange("p (n h) -> p n h", h=NH),
                                             in1=bias_bc[:].unsqueeze(1).broadcast_to([128, NT, NH]), op=ALU.add),
         reads=[pk, bias_key], writes=[tmp_key])
    P.op("scalar", lambda e: e.activation(out=tmp[:], in_=tmp[:], func=AF.Exp, scale=-1.0), reads=[tmp_key], writes=[tmp_key])
    P.op("scalar", lambda e: e.activation(out=tmp[:], in_=tmp[:], func=AF.Ln, bias=1.0), reads=[tmp_key], writes=[tmp_key])
    P.op("vector", lambda e: e.tensor_scalar(out=out[:], in0=tmp[:], scalar1=-1.0, scalar2=None, op0=ALU.mult),
         reads=[tmp_key], writes=[out_key])


def proj_fm(C, xnT, L, w_view, col0, ncol_chunks, outT, out_key, wrot, post=None):
    P = C.P
    for b0 in range(0, ncol_chunks, 4):
        nb = min(4, ncol_chunks - b0)
        wt, wk = wrot.next()
        P.dma("gpsimd", wt[:, :, 0:nb * 128], w_view[:, :, col0 + b0 * 128: col0 + (b0 + nb) * 128], writes=[wk], semkey=wk)
        for cc in range(b0, b0 + nb):
            for tb in range(L // 512):
                ps, pk = C.pA.next()
                for c in range(8):
                    P.op("tensor", lambda e, c=c, ps=ps, wt=wt, cc=cc, tb=tb, b0=b0: e.matmul(
                        out=ps[:], lhsT=wt[:, c, (cc - b0) * 128:(cc - b0 + 1) * 128], rhs=xnT[:, c, tb * 512:(tb + 1) * 512],
                        start=(c == 0), stop=(c == 7)),
                        reads=[wk] + [("xnT", tb * 4 + q) for q in range(4)], writes=[pk])
                if post is None:
                    C.evac(outT[:, cc, tb * 512:(tb + 1) * 512], ps[:], [pk], [(out_key, cc, tb)])
                else:
                    post(ps, pk, cc, tb)


def stage_fox(C, hsrc, hdst, tile0, L, W, li, osc):
    P = C.P
    NT = L // 128
    NH, DH = 16, 64
    HD = NH * DH
    w_in = W["fox_w_in"][li]
    w_view = w_in.rearrange("(c p) n -> p c n", p=128)
    P.push()
    xnT = P.sb([128, 8, L], BF16, "f_xnT")
    load_gain(C, W["fox_norm_g"][li:li + 1, :])
    norm_T(C, hsrc, tile0, NT, xnT, "xnT")
    xkeys = [("xnT", n) for n in range(NT)]
    P.push()
    lf = P.sb([128, NT, NH], F32, "f_lf")
    tmpg = P.sb([128, NT, NH], F32, "f_tmpg")
    bfb = P.sb([128, NH], F32, "f_bfb")
    wf = P.sb([128, 8, NH], BF16, "f_wf")
    P.dma("sync", bfb[:], W["fox_b_f"][li:li + 1, :].broadcast_to([128, NH]), writes=["bfb"], semkey="f_bfb")
    P.dma("gpsimd", wf[:], w_view[:, :, 3 * HD:3 * HD + NH], writes=["wf"], semkey="f_wf")
    psg, pkg = C.pA.next()
    for n in range(NT):
        for c in range(8):
            P.op("tensor", lambda e, n=n, c=c: e.matmul(out=psg[:, n * NH:(n + 1) * NH], lhsT=xnT[:, c, n * 128:(n + 1) * 128],
                                                        rhs=wf[:, c, :], start=(c == 0), stop=(c == 7)),
                 reads=["wf", ("xnT", n)], writes=[pkg])
    log_sigmoid_tok(C, psg, pkg, bfb, "bfb", lf, "f_lf", NT, NH, tmpg, "tmpg")
    cum, carry, tot = gate_cumsum(C, lf, NT, NH, "f_")
    Bq = P.sb([128, NT, NH], F32, "f_Bq")
    P.op("vector", lambda e: e.scalar_tensor_tensor(out=Bq[:], in0=tot[:], scalar=0.5, in1=carry[:, 0:NT, :],
                                                    op0=ALU.mult, op1=ALU.add),
         reads=["f_tot", "f_carry"], writes=["Bq"])
    GH = 8
    qT = P.sb([128, 4, L], BF16, "f_qT")
    kT = P.sb([128, 4, L], BF16, "f_kT")
    vaug = P.sb([128, NT, GH, DH + 1], BF16, "f_vaug")
    wrot = Rot([(P.sb([128, 8, 512], BF16, f"f_w{i}"), f"f_w{i}") for i in range(2)])
    biasr = Rot([(P.sb([128, NT, GH], F32, f"f_bias{i}"), f"f_bias{i}") for i in range(2)])
    ptr = Rot([(P.sb([128, 4, 128], BF16, f"f_pt{i}"), f"f_pt{i}") for i in range(3)])
    obr = Rot([(P.sb([128, GH * DH], BF16, f"f_ob{i}"), f"f_ob{i}") for i in range(2)])
    rdr = Rot([(P.sb([128, 1], F32, f"f_rd{i}"), f"f_rd{i}") for i in range(4)])
    po = P.ps([128, 2, 512], F32, "f_po")
    poi = 0
    P.op("gpsimd", lambda e: e.memset(vaug[:, :, :, DH:DH + 1], 1.0), writes=["vones"])
    for g in range(NH // GH):
        proj_fm(C, xnT, L, w_view, g * GH * DH, 4, qT, "qT", wrot)
        proj_fm(C, xnT, L, w_view, HD + g * GH * DH, 4, kT, "kT", wrot)
        wt, wk = wrot.next()
        P.dma("gpsimd", wt[:], w_view[:, :, 2 * HD + g * GH * DH: 2 * HD + (g + 1) * GH * DH], writes=[wk], semkey=wk)
        for n in range(NT):
            ps, pk = C.pA.next()
            for c in range(8):
                P.op("tensor", lambda e, n=n, c=c, ps=ps, wt=wt: e.matmul(out=ps[:], lhsT=xnT[:, c, n * 128:(n + 1) * 128],
                                                                      rhs=wt[:, c, :], start=(c == 0), stop=(c == 7)),
                     reads=[wk, ("xnT", n)], writes=[pk])
            C.evac(vaug[:, n, :, 0:DH], ps[:].rearrange("p (h d) -> p h d", d=DH), [pk], [("v", n)])
        for Q in range(NT):
            bq, bk = biasr.next()
            P.op("vector", lambda e, bq=bq, Q=Q, g=g: e.tensor_tensor(
                out=bq[:, 0:Q + 1, :], in0=Bq[:, Q:Q + 1, g * GH:(g + 1) * GH].broadcast_to([128, Q + 1, GH]),
                in1=cum[:, 0:Q + 1, g * GH:(g + 1) * GH], op=ALU.subtract),
                reads=["Bq", "f_cum"], writes=[bk])
            ob, obk = obr.next()
            for hh in range(GH):
                c, half = hh // 2, hh % 2
                p0 = half * 64
                slot = poi % 2
                poi += 1
                for J0 in range(0, Q + 1, 4):
                    nj = min(4, Q + 1 - J0)
                    ps, pk = C.pA.next()
                    pt, ptk = ptr.next()
                    for jj in range(nj):
                        J = J0 + jj
                        P.op("tensor", lambda e, ps=ps, jj=jj, J=J, c=c, p0=p0, Q=Q: e.matmul(
                            out=ps[:, jj * 128:(jj + 1) * 128], lhsT=kT[p0:p0 + 64, c, J * 128:(J + 1) * 128],
                            rhs=qT[p0:p0 + 64, c, Q * 128:(Q + 1) * 128], start=True, stop=True),
                            reads=[("kT", c, J // 4), ("qT", c, Q // 4)], writes=[pk])
                    for jj in range(nj):
                        J = J0 + jj
                        P.op("scalar", lambda e, ps=ps, pt=pt, jj=jj, J=J, bq=bq, hh=hh: e.activation(
                            out=pt[:, jj, :], in_=ps[:, jj * 128:(jj + 1) * 128], func=AF.Exp, scale=0.125,
                            bias=bq[:, J, hh:hh + 1]),
                            reads=[pk, bk], writes=[(ptk, jj)])
                        if J == Q:
                            P.op("gpsimd", lambda e, pt=pt, jj=jj: e.tensor_tensor(out=pt[:, jj, :], in0=pt[:, jj, :],
                                                                                   in1=C.mask01[:], op=ALU.mult),
                                 reads=[(ptk, jj), "mask01"], writes=[(ptk, jj)])
                        P.op("tensor", lambda e, pt=pt, jj=jj, J=J, hh=hh, slot=slot, Q=Q: e.matmul(
                            out=po[:, slot, 0:DH + 1], lhsT=pt[:, jj, :], rhs=vaug[:, J, hh, :], start=(J == 0), stop=(J == Q)),
                            reads=[(ptk, jj), ("v", J), "vones"], writes=[("po", slot)])
                rd, rdk = rdr.next()
                P.op("vector", lambda e, rd=rd, slot=slot: e.reciprocal(out=rd[:], in_=po[:, slot, DH:DH + 1]),
                     reads=[("po", slot)], writes=[rdk])
                P.op("vector", lambda e, rd=rd, slot=slot, ob=ob, hh=hh: e.tensor_scalar(
                    out=ob[:, hh * DH:(hh + 1) * DH], in0=po[:, slot, 0:DH], scalar1=rd[:, 0:1], scalar2=None, op0=ALU.mult),
                    reads=[("po", slot), rdk], writes=[obk])
            tile = tile0 + Q
            P.dma("sync", osc[tile * 128:(tile + 1) * 128, g * GH * DH:(g + 1) * GH * DH], ob[:],
                  reads=[obk], writes=[("osc", tile, g)], semkey=obk)
    P.pop()
    mixer_out(C, hsrc, hdst, tile0, NT, xnT, w_view[:, :, 3 * HD + NH: 3 * HD + NH + HD], W["fox_w_out"][li], osc, 2, "fo_")
    P.pop()


def mixer_out(C, hsrc, hdst, tile0, NT, xnT, wg_view, w_out, osc, ngroups, pfx, pre=None):
    P = C.P
    P.push()
    wg = P.sb([128, 8, D], BF16, pfx + "wg")
    wo = P.sb([128, 8, D], BF16, pfx + "wo")
    P.dma("gpsimd", wg[:, :, 0:512], wg_view[:, :, 0:512], writes=["wg"], semkey=pfx + "wg")
    P.dma("gpsimd", wg[:, :, 512:1024], wg_view[:, :, 512:1024], writes=["wg"], semkey=pfx + "wg")
    wo_view = w_out.rearrange("(c p) n -> p c n", p=128)
    P.dma("gpsimd", wo[:, :, 0:512], wo_view[:, :, 0:512], writes=["wo"], semkey=pfx + "wo")
    P.dma("gpsimd", wo[:, :, 512:1024], wo_view[:, :, 512:1024], writes=["wo"], semkey=pfx + "wo")
    otr = Rot([(P.sb([128, D], BF16, pfx + f"ot{i}"), pfx + f"ot{i}") for i in range(2)])
    sgr = Rot([(P.sb([128, D], F32, pfx + f"sg{i}"), pfx + f"sg{i}") for i in range(2)])
    ogr = Rot([(P.sb([128, D], BF16, pfx + f"og{i}"), pfx + f"og{i}") for i in range(2)])
    ogTr = Rot([(P.sb([128, 8, 128], BF16, pfx + f"ogT{i}"), pfx + f"ogT{i}") for i in range(2)])
    for n in range(NT):
        tile = tile0 + n
        ot, otk = otr.next()
        P.dma("sync", ot[:], osc[tile * 128:(tile + 1) * 128, :], reads=[("osc", tile, g) for g in range(ngroups)],
              writes=[otk], semkey=otk)
        sg, sgk = sgr.next()
        for hf in range(2):
            ps, pk = C.pA.next()
            for c in range(8):
                P.op("tensor", lambda e, c=c, ps=ps, n=n, hf=hf: e.matmul(
                    out=ps[:], lhsT=xnT[:, c, n * 128:(n + 1) * 128], rhs=wg[:, c, hf * 512:(hf + 1) * 512],
                    start=(c == 0), stop=(c == 7)), reads=["wg", ("xnT", n)], writes=[pk])
            P.op("scalar", lambda e, ps=ps, sg=sg, hf=hf: e.activation(out=sg[:, hf * 512:(hf + 1) * 512], in_=ps[:], func=AF.Sigmoid),
                 reads=[pk], writes=[(sgk, hf)])
        og, ogk = ogr.next()
        if pre is not None:
            pre(n, ot, otk)
        P.op("vector", lambda e, og=og, ot=ot, sg=sg: e.tensor_tensor(out=og[:], in0=ot[:], in1=sg[:], op=ALU.mult),
             reads=[otk, (sgk, 0), (sgk, 1)], writes=[ogk])
        pT, ptk = C.pT.next()
        for c in range(8):
            P.op("tensor", lambda e, c=c, pT=pT, og=og: e.transpose(out=pT[:, c, :], in_=og[:, c * 128:(c + 1) * 128],
                                                                identity=C.ident[:]),
                 reads=[ogk, "ident"], writes=[ptk])
        ogT, ogTk = ogTr.next()
        C.evac(ogT[:], pT[:], [ptk], [ogTk])
        ht, hk = C.ht.next()
        P.dma("sync", ht[:], hsrc[tile * 128:(tile + 1) * 128, :], reads=hkeys(tile), writes=[hk], semkey=hk)
        for hf in range(2):
            ps, pk = C.pA.next()
            for c in range(8):
                P.op("tensor", lambda e, c=c, ps=ps, ogT=ogT, hf=hf: e.matmul(
                    out=ps[:], lhsT=ogT[:, c, :], rhs=wo[:, c, hf * 512:(hf + 1) * 512], start=(c == 0), stop=(c == 7)),
                    reads=["wo", ogTk], writes=[pk])
            P.op("vector", lambda e, ps=ps, ht=ht, hf=hf: e.tensor_tensor(out=ht[:, hf * 512:(hf + 1) * 512],
                                                                        in0=ht[:, hf * 512:(hf + 1) * 512], in1=ps[:], op=ALU.add),
                 reads=[pk, hk], writes=[hk])
        P.dma("sync", hdst[tile * 128:(tile + 1) * 128, :], ht[:], reads=[hk], writes=hkeys(tile), semkey=hk)
    P.pop()


def stage_ml(C, hsrc, hdst, tile0, L, W, li, osc):
    P = C.P
    NT = L // 128
    NH, DK, DV = 8, 64, 128
    w_in = W["ml_w_in"][li]
    w_view = w_in.rearrange("(c p) n -> p c n", p=128)
    O0, O1 = 2 * NH * DK, 2 * NH * DK + NH * DV
    P.push()
    xnT = P.sb([128, 8, L], BF16, "l_xnT")
    load_gain(C, W["ml_norm_g"][li:li + 1, :])
    norm_T(C, hsrc, tile0, NT, xnT, "xnT")
    P.push()
    cw5 = P.sb([5, 1024], F32, "l_cw5")
    cw = P.sb([128, 8, 5], F32, "l_cw")
    P.dma("sync", cw5[0:4, :], W["ml_conv_w"][li], writes=["cw5a"], semkey="l_cw5a")
    P.dma("sync", cw5[4:5, :], W["ml_conv_b"][li:li + 1, :], writes=["cw5b"], semkey="l_cw5b")
    psc, pkc = C.pA.next()
    for c in range(8):
        P.op("tensor", lambda e, c=c: e.transpose(out=psc[:, c * 5:(c + 1) * 5], in_=cw5[0:5, c * 128:(c + 1) * 128],
                                                  identity=C.identf[0:5, 0:5]),
             reads=["cw5a", "cw5b", "identf"], writes=[pkc])
    P.op("vector", lambda e: e.tensor_copy(out=cw[:].rearrange("p c k -> p (c k)"), in_=psc[:, 0:40]), reads=[pkc], writes=["cw"])
    lf = P.sb([128, NT, NH], F32, "l_lf")
    lig = P.sb([128, NT, NH], F32, "l_li")
    tmpg = P.sb([128, NT, NH], F32, "l_tmpg")
    bfb = P.sb([128, 2 * NH], F32, "l_bfb")
    wg2 = P.sb([128, 8, 2 * NH], BF16, "l_wg2")
    P.dma("sync", bfb[:, 0:NH], W["ml_b_i"][li:li + 1, :].broadcast_to([128, NH]), writes=["bfb"], semkey="l_bfb")
    P.dma("sync", bfb[:, NH:2 * NH], W["ml_b_f"][li:li + 1, :].broadcast_to([128, NH]), writes=["bfb"], semkey="l_bfb")
    P.dma("gpsimd", wg2[:], w_view[:, :, O1:O1 + 2 * NH], writes=["wg2"], semkey="l_wg2")
    psi, pki = C.pA.next()
    psf, pkf = C.pA.next()
    for n in range(NT):
        for (pp, kk, off) in ((psi, pki, 0), (psf, pkf, NH)):
            for c in range(8):
                P.op("tensor", lambda e, n=n, c=c, pp=pp, off=off: e.matmul(
                    out=pp[:, n * NH:(n + 1) * NH], lhsT=xnT[:, c, n * 128:(n + 1) * 128], rhs=wg2[:, c, off:off + NH],
                    start=(c == 0), stop=(c == 7)), reads=["wg2", ("xnT", n)], writes=[kk])
    P.op("vector", lambda e: e.tensor_tensor(out=lig[:], in0=psi[:, 0:NT * NH].rearrange("p (n h) -> p n h", h=NH),
                                             in1=bfb[:, 0:NH].unsqueeze(1).broadcast_to([128, NT, NH]), op=ALU.add),
         reads=[pki, "bfb"], writes=["l_li"])
    log_sigmoid_tok(C, psf, pkf, bfb[:, NH:2 * NH], "bfb", lf, "l_lf", NT, NH, tmpg, "tmpg")
    cum, carry, tot = gate_cumsum(C, lf, NT, NH, "l_")
    Bq = P.sb([128, NT, NH], F32, "l_Bq")
    P.op("vector", lambda e: e.scalar_tensor_tensor(out=Bq[:], in0=tot[:], scalar=0.5, in1=carry[:, 0:NT, :],
                                                    op0=ALU.mult, op1=ALU.add),
         reads=["l_tot", "l_carry"], writes=["Bq"])
    cml = P.sb([128, NT, NH], F32, "l_cml")
    Fr = P.sb([128, NT, NH], F32, "l_Fr")
    P.op("vector", lambda e: e.tensor_tensor(out=cml[:], in0=cum[:], in1=lig[:], op=ALU.subtract),
         reads=["l_cum", "l_li"], writes=["cml"])
    P.op("vector", lambda e: e.tensor_tensor(out=Fr[:], in0=cum[:], in1=Bq[:], op=ALU.subtract), reads=["l_cum", "Bq"], writes=["Fr"])
    P.op("scalar", lambda e: e.activation(out=Fr[:], in_=Fr[:], func=AF.Exp), reads=["Fr"], writes=["Fr"])
    qT = P.sb([128, 4, L], BF16, "l_qT")
    kT = P.sb([128, 4, L], BF16, "l_kT")
    zpr = Rot([(P.sb([128, L + 3], F32, f"l_zp{i}"), f"l_zp{i}") for i in range(2)])
    accr = Rot([(P.sb([128, L], F32, f"l_acc{i}"), f"l_acc{i}") for i in range(2)])
    wrot = Rot([(P.sb([128, 8, 512], BF16, f"l_w{i}"), f"l_w{i}") for i in range(2)])
    for zp, zk in zpr.items:
        P.op("vector", lambda e, zp=zp: e.memset(zp[:, 0:3], 0.0), writes=[(zk, "pad")])
    for which, dst, dkey in ((0, qT, "qT"), (1, kT, "kT")):
        state = {}

        def post(ps, pk, cc, tb, which=which, dst=dst, dkey=dkey, state=state):
            if tb == 0:
                state["zp"] = zpr.next()
            zp, zk = state["zp"]
            C.evac(zp[:, 3 + tb * 512: 3 + (tb + 1) * 512], ps[:], [pk], [(zk, tb)])
            if tb == L // 512 - 1:
                fc = which * 4 + cc
                acc, ak = accr.next()
                zkeys = [(zk, "pad")] + [(zk, t) for t in range(L // 512)]
                P.op("vector", lambda e: e.tensor_scalar(out=acc[:], in0=zp[:, 3:L + 3], scalar1=cw[:, fc, 3:4],
                                                         scalar2=cw[:, fc, 4:5], op0=ALU.mult, op1=ALU.add),
                     reads=zkeys + ["cw"], writes=[ak])
                for k in (2, 1, 0):
                    P.op("vector", lambda e, k=k: e.scalar_tensor_tensor(out=acc[:], in0=zp[:, k:L + k], scalar=cw[:, fc, k:k + 1],
                                                                      in1=acc[:], op0=ALU.mult, op1=ALU.add),
                         reads=zkeys + ["cw", ak], writes=[ak])
                P.op("scalar", lambda e: e.activation(out=dst[:, cc, :], in_=acc[:], func=AF.Silu), reads=[ak],
                     writes=[(dkey, cc, t) for t in range(L // 512)])
        proj_fm(C, xnT, L, w_view, which * NH * DK, 4, None, None, wrot, post=post)
    vaug = P.sb([128, NT, NH, DV + 1], BF16, "l_vaug")
    P.op("gpsimd", lambda e: e.memset(vaug[:, :, :, DV:DV + 1], 1.0), writes=["vones"])
    for hf in range(2):
        wt, wk = wrot.next()
        P.dma("gpsimd", wt[:], w_view[:, :, O0 + hf * 512: O0 + (hf + 1) * 512], writes=[wk], semkey=wk)
        for n in range(NT):
            ps, pk = C.pA.next()
            for c in range(8):
                P.op("tensor", lambda e, n=n, c=c, ps=ps, wt=wt: e.matmul(out=ps[:], lhsT=xnT[:, c, n * 128:(n + 1) * 128],
                                                                      rhs=wt[:, c, :], start=(c == 0), stop=(c == 7)),
                     reads=[wk, ("xnT", n)], writes=[pk])
            C.evac(vaug[:, n, hf * 4:(hf + 1) * 4, 0:DV], ps[:].rearrange("p (h d) -> p h d", d=DV), [pk], [("v", n, hf)])
    hgb = P.sb([128, D], F32, "l_hgb")
    P.dma("sync", hgb[:], W["ml_head_g"][li:li + 1, :].broadcast_to([128, D]), writes=["hgb"], semkey="l_hgb")
    Er = Rot([(P.sb([128, NT, NH], F32, f"l_E{i}"), f"l_E{i}") for i in range(2)])
    ptr = Rot([(P.sb([128, 4, 128], BF16, f"l_pt{i}"), f"l_pt{i}") for i in range(3)])
    obr = Rot([(P.sb([128, NH, DV], F32, f"l_ob{i}"), f"l_ob{i}") for i in range(2)])
    obbr = Rot([(P.sb([128, D], BF16, f"l_obb{i}"), f"l_obb{i}") for i in range(2)])
    sqt = P.sb([128, NH, DV], F32, "l_sqt")
    st = Rot([(P.sb([128, 4, NH], F32, f"l_st{i}"), f"l_st{i}") for i in range(2)])
    cfr = Rot([(P.sb([128, 2], F32, f"l_cf{i}"), f"l_cf{i}") for i in range(4)])
    po = P.ps([128, 2, 512], F32, "l_po")
    poi = 0
    for Q in range(NT):
        E, Ek = Er.next()
        P.op("vector", lambda e, E=E, Q=Q: e.tensor_tensor(
            out=E[:, 0:Q + 1, :], in0=Bq[:, Q:Q + 1, :].broadcast_to([128, Q + 1, NH]), in1=cml[:, 0:Q + 1, :], op=ALU.subtract),
            reads=["Bq", "cml"], writes=[Ek])
        P.op("scalar", lambda e, E=E, Q=Q: e.activation(out=E[:, 0:Q + 1, :], in_=E[:, 0:Q + 1, :], func=AF.Exp, bias=-2.0794415416798357),
             reads=[Ek], writes=[Ek])
        ob, obk = obr.next()
        for hh in range(NH):
            c, half = hh // 2, hh % 2
            p0 = half * 64
            slot = poi % 2
            poi += 1
            for J0 in range(0, Q + 1, 4):
                nj = min(4, Q + 1 - J0)
                ps, pk = C.pA.next()
                pt, ptk = ptr.next()
                for jj in range(nj):
                    J = J0 + jj
                    P.op("tensor", lambda e, ps=ps, jj=jj, J=J, c=c, p0=p0, Q=Q: e.matmul(
                        out=ps[:, jj * 128:(jj + 1) * 128], lhsT=kT[p0:p0 + 64, c, J * 128:(J + 1) * 128],
                        rhs=qT[p0:p0 + 64, c, Q * 128:(Q + 1) * 128], start=True, stop=True),
                        reads=[("kT", c, J // 4), ("qT", c, Q // 4)], writes=[pk])
                for jj in range(nj):
                    J = J0 + jj
                    eng = "vector"
                    if eng == "vector":
                        P.op("vector", lambda e, ps=ps, pt=pt, jj=jj, J=J, E=E, hh=hh: e.tensor_scalar(
                            out=pt[:, jj, :], in0=ps[:, jj * 128:(jj + 1) * 128], scalar1=E[:, J, hh:hh + 1], scalar2=None,
                            op0=ALU.mult), reads=[pk, Ek], writes=[(ptk, jj)])
                    else:
                        P.op("scalar", lambda e, ps=ps, pt=pt, jj=jj, J=J, E=E, hh=hh: e.activation(
                            out=pt[:, jj, :], in_=ps[:, jj * 128:(jj + 1) * 128], func=AF.Copy, scale=E[:, J, hh:hh + 1]),
                            reads=[pk, Ek], writes=[(ptk, jj)])
                        P.op("vector", lambda e, pt=pt, jj=jj: e.tensor_scalar(out=pt[:, jj, :], in0=pt[:, jj, :], scalar1=0.125,
                                                                               scalar2=None, op0=ALU.mult),
                             reads=[(ptk, jj)], writes=[(ptk, jj)])
                    if J == Q:
                        P.op("gpsimd", lambda e, pt=pt, jj=jj: e.tensor_tensor(out=pt[:, jj, :], in0=pt[:, jj, :],
                                                                               in1=C.mask01[:], op=ALU.mult),
                             reads=[(ptk, jj), "mask01"], writes=[(ptk, jj)])
                    P.op("tensor", lambda e, pt=pt, jj=jj, J=J, hh=hh, slot=slot, Q=Q: e.matmul(
                        out=po[:, slot, 0:DV + 1], lhsT=pt[:, jj, :], rhs=vaug[:, J, hh, :], start=(J == 0), stop=(J == Q)),
                        reads=[(ptk, jj), ("v", J, hh // 4), "vones"], writes=[("po", slot)])
            cf, cfk = cfr.next()
            P.op("scalar", lambda e, cf=cf, slot=slot, Q=Q, hh=hh: e.activation(
                out=cf[:, 0:1], in_=po[:, slot, DV:DV + 1], func=AF.Abs, scale=Fr[:, Q, hh:hh + 1]),
                reads=[("po", slot), "Fr"], writes=[cfk])
            P.op("vector", lambda e, cf=cf: e.tensor_scalar(out=cf[:, 0:1], in0=cf[:, 0:1], scalar1=1.0, scalar2=None, op0=ALU.max),
                 reads=[cfk], writes=[cfk])
            P.op("vector", lambda e, cf=cf: e.reciprocal(out=cf[:, 1:2], in_=cf[:, 0:1]), reads=[cfk], writes=[cfk])
            P.op("vector", lambda e, cf=cf, slot=slot, ob=ob, hh=hh, Q=Q: e.tensor_scalar(
                out=ob[:, hh, :], in0=po[:, slot, 0:DV], scalar1=cf[:, 1:2], scalar2=Fr[:, Q, hh:hh + 1], op0=ALU.mult, op1=ALU.mult),
                reads=[("po", slot), cfk, "Fr"], writes=[obk])
        s4, sk4 = st.next()
        P.op("vector", lambda e, s4=s4, ob=ob: e.tensor_reduce(out=s4[:, 0, :], in_=ob[:], axis=AX.X, op=ALU.add), reads=[obk], writes=[sk4])
        P.op("scalar", lambda e, ob=ob: e.activation(out=sqt[:], in_=ob[:], func=AF.Square), reads=[obk], writes=["sqt"])
        P.op("vector", lambda e, s4=s4: e.tensor_reduce(out=s4[:, 1, :], in_=sqt[:], axis=AX.X, op=ALU.add), reads=["sqt", sk4], writes=[sk4])
        P.op("vector", lambda e, s4=s4: e.tensor_scalar(out=s4[:, 0:2, :], in0=s4[:, 0:2, :], scalar1=1.0 / DV, scalar2=None, op0=ALU.mult),
             reads=[sk4], writes=[sk4])
        P.op("vector", lambda e, s4=s4: e.tensor_tensor(out=s4[:, 2, :], in0=s4[:, 0, :], in1=s4[:, 0, :], op=ALU.mult), reads=[sk4], writes=[sk4])
        P.op("vector", lambda e, s4=s4: e.tensor_tensor(out=s4[:, 2, :], in0=s4[:, 1, :], in1=s4[:, 2, :], op=ALU.subtract), reads=[sk4], writes=[sk4])
        P.op("scalar", lambda e, s4=s4: e.activation(out=s4[:, 2, :], in_=s4[:, 2, :], func=AF.Sqrt, bias=1e-6), reads=[sk4], writes=[sk4])
        P.op("vector", lambda e, s4=s4: e.reciprocal(out=s4[:, 3, :], in_=s4[:, 2, :]), reads=[sk4], writes=[sk4])
        P.op("vector", lambda e, s4=s4, ob=ob: e.tensor_tensor(out=ob[:], in0=ob[:], in1=s4[:, 0, :].unsqueeze(2).broadcast_to([128, NH, DV]),
                                                             op=ALU.subtract), reads=[obk, sk4], writes=[obk])
        P.op("vector", lambda e, s4=s4, ob=ob: e.tensor_tensor(out=ob[:], in0=ob[:], in1=s4[:, 3, :].unsqueeze(2).broadcast_to([128, NH, DV]),
                                                             op=ALU.mult), reads=[obk, sk4], writes=[obk])
        obb, obbk = obbr.next()
        P.op("vector", lambda e, ob=ob, obb=obb: e.tensor_tensor(out=obb[:], in0=ob[:].rearrange("p h d -> p (h d)"), in1=hgb[:], op=ALU.mult),
             reads=[obk, "hgb"], writes=[obbk])
        tile = tile0 + Q
        P.dma("sync", osc[tile * 128:(tile + 1) * 128, :], obb[:], reads=[obbk], writes=[("osc", tile, 0)], semkey=obbk)
    P.pop()
    mixer_out(C, hsrc, hdst, tile0, NT, xnT, w_view[:, :, O1 + 2 * NH: O1 + 2 * NH + D], W["ml_w_out"][li], osc, 1, "lo_")
    P.pop()


TWO_PI = 6.283185307179586
PI = 3.141592653589793


def range_reduce(P, x, ki, kf, out_r, rk, wk, tk):
    P.op("vector", lambda e: e.tensor_scalar(out=ki, in0=x, scalar1=1.0 / TWO_PI, scalar2=None, op0=ALU.mult), reads=rk, writes=[tk + "i"])
    P.op("vector", lambda e: e.tensor_copy(out=kf, in_=ki), reads=[tk + "i"], writes=[tk + "f"])
    P.op("vector", lambda e: e.scalar_tensor_tensor(out=kf, in0=kf, scalar=-TWO_PI, in1=x, op0=ALU.mult, op1=ALU.add),
         reads=[tk + "f"] + list(rk), writes=[tk + "f"])
    P.op("vector", lambda e: e.tensor_scalar(out=out_r, in0=kf, scalar1=PI, scalar2=-PI, op0=ALU.min, op1=ALU.max),
         reads=[tk + "f"], writes=wk)


def sincos(P, r, out_s, out_c, rk, wk_s, wk_c):
    P.op("scalar", lambda e: e.activation(out=out_c, in_=r, func=AF.Abs), reads=rk, writes=wk_c)
    P.op("scalar", lambda e: e.activation(out=out_s, in_=r, func=AF.Sin), reads=rk, writes=wk_s)
    P.op("scalar", lambda e: e.activation(out=out_c, in_=out_c, func=AF.Sin, scale=-1.0, bias=PI / 2), reads=wk_c, writes=wk_c)


def stage_s5(C, hsrc, hdst, tile0, L, W, li, tabs, first):
    P = C.P
    NT = L // 128
    TBK = min(L, 1024)
    NBK = L // TBK
    G2 = 32
    P.push()
    xnT = P.sb([128, 8, L], BF16, "s_xnT")
    load_gain(C, W["s5_norm_g"][li:li + 1, :])
    norm_T(C, hsrc, tile0, NT, xnT, "xnT")
    xk_all = [("xnT", n) for n in range(NT)]
    P.push()
    PA = P.sb([32, 3, 128], F32, "s_PA")
    ldt = P.sb([32, 2], F32, "s_ldt")
    P.dma("sync", PA[:, 0, :], W["s5_a_re"][li].rearrange("(gp g2) p -> gp (g2 p)", g2=2), writes=["PA0"], semkey="s_PA0")
    P.dma("sync", PA[:, 1, :], W["s5_a_im"][li].rearrange("(gp g2) p -> gp (g2 p)", g2=2), writes=["PA1"], semkey="s_PA1")
    P.dma("sync", ldt[:], W["s5_log_dt"][li].rearrange("(gp g2) -> gp g2", g2=2), writes=["ldt"], semkey="s_ldt")
    P.op("vector", lambda e: e.tensor_copy(out=PA[:, 2, :].rearrange("q (g p) -> q g p", p=64),
                                           in_=ldt[:].unsqueeze(2).broadcast_to([32, 2, 64])), reads=["ldt"], writes=["PA2"])
    psp, pkp = C.pA.next()
    for k in range(3):
        P.op("tensor", lambda e, k=k: e.transpose(out=psp[:, k * 32:(k + 1) * 32], in_=PA[:, k, :], identity=C.identf[0:32, 0:32]),
             reads=["PA0", "PA1", "PA2", "identf"], writes=[pkp])
    prm = P.sb([128, 14, G2], F32, "s_prm")
    pki = P.sb([128, G2], mybir.dt.int32, "s_pki")
    P.op("vector", lambda e: e.tensor_copy(out=prm[:, 0:3, :].rearrange("q k g -> q (k g)"), in_=psp[:, 0:96]), reads=[pkp], writes=["prm"])
    AR, AI, LDT, DT, RM, ANG, SINA, COSA, ABR, ABI, CRE, CIM, T1, T2 = [prm[:, k, :] for k in range(14)]

    def vv(fn, **kw):
        P.op("vector", fn, reads=["prm"], writes=["prm"])

    def aa(fn):
        P.op("scalar", fn, reads=["prm"], writes=["prm"])
    aa(lambda e: e.activation(out=DT, in_=LDT, func=AF.Exp))
    vv(lambda e: e.tensor_tensor(out=RM, in0=DT, in1=AR, op=ALU.mult))
    aa(lambda e: e.activation(out=RM, in_=RM, func=AF.Exp))
    vv(lambda e: e.tensor_tensor(out=T1, in0=DT, in1=AI, op=ALU.mult))
    range_reduce(P, T1, pki[:], T2, ANG, ["prm"], ["prm"], "s_rr0")
    sincos(P, ANG, SINA, COSA, ["prm", "s_rr0f"], ["prm"], ["prm"])
    vv(lambda e: e.tensor_tensor(out=ABR, in0=RM, in1=COSA, op=ALU.mult))
    vv(lambda e: e.tensor_tensor(out=ABI, in0=RM, in1=SINA, op=ALU.mult))
    vv(lambda e: e.tensor_scalar(out=ABR, in0=ABR, scalar1=-1.0, scalar2=None, op0=ALU.add))
    vv(lambda e: e.tensor_tensor(out=T1, in0=AR, in1=AR, op=ALU.mult))
    vv(lambda e: e.tensor_tensor(out=T2, in0=AI, in1=AI, op=ALU.mult))
    vv(lambda e: e.tensor_tensor(out=T1, in0=T1, in1=T2, op=ALU.add))
    vv(lambda e: e.reciprocal(out=T1, in_=T1))
    vv(lambda e: e.tensor_tensor(out=CRE, in0=ABR, in1=AR, op=ALU.mult))
    vv(lambda e: e.tensor_tensor(out=T2, in0=ABI, in1=AI, op=ALU.mult))
    vv(lambda e: e.tensor_tensor(out=CRE, in0=CRE, in1=T2, op=ALU.add))
    vv(lambda e: e.tensor_tensor(out=CRE, in0=CRE, in1=T1, op=ALU.mult))
    vv(lambda e: e.tensor_tensor(out=CIM, in0=ABI, in1=AR, op=ALU.mult))
    vv(lambda e: e.tensor_tensor(out=T2, in0=ABR, in1=AI, op=ALU.mult))
    vv(lambda e: e.tensor_tensor(out=CIM, in0=CIM, in1=T2, op=ALU.subtract))
    vv(lambda e: e.tensor_tensor(out=CIM, in0=CIM, in1=T1, op=ALU.mult))
    thb = P.sb([128, NBK, G2], F32, "s_thb")
    for b in range(NBK):
        P.op("vector", lambda e, b=b: e.tensor_scalar(out=thb[:, b, :], in0=ANG, scalar1=float(b * TBK), scalar2=None, op0=ALU.mult),
             reads=["prm"], writes=["thb"])
    bre = P.sb([128, G2, 16], F32, "s_bre")
    bim = P.sb([128, G2, 16], F32, "s_bim")
    bbr = P.sb([128, G2, 16], F32, "s_bbr")
    bbi = P.sb([128, G2, 16], F32, "s_bbi")
    btmp = P.sb([128, G2, 16], F32, "s_btmp")
    for q4 in range(4):
        P.dma("sync", bre[:, q4 * 8:(q4 + 1) * 8, :], W["s5_b_re"][li].rearrange("(gp g2) p h -> (g2 p) gp h", g2=2)[:, q4 * 8:(q4 + 1) * 8, :],
              writes=["bre"], semkey="s_bre")
        P.dma("sync", bim[:, q4 * 8:(q4 + 1) * 8, :], W["s5_b_im"][li].rearrange("(gp g2) p h -> (g2 p) gp h", g2=2)[:, q4 * 8:(q4 + 1) * 8, :],
              writes=["bim"], semkey="s_bim")
    cre_b = CRE.unsqueeze(2).broadcast_to([128, G2, 16])
    cim_b = CIM.unsqueeze(2).broadcast_to([128, G2, 16])
    P.op("vector", lambda e: e.tensor_tensor(out=bbr[:], in0=bre[:], in1=cre_b, op=ALU.mult), reads=["prm", "bre"], writes=["bbr"])
    P.op("vector", lambda e: e.tensor_tensor(out=btmp[:], in0=bim[:], in1=cim_b, op=ALU.mult), reads=["prm", "bim"], writes=["btmp"])
    P.op("vector", lambda e: e.tensor_tensor(out=bbr[:], in0=bbr[:], in1=btmp[:], op=ALU.subtract), reads=["bbr", "btmp"], writes=["bbr"])
    P.op("vector", lambda e: e.tensor_tensor(out=bbi[:], in0=bim[:], in1=cre_b, op=ALU.mult), reads=["prm", "bim"], writes=["bbi"])
    P.op("vector", lambda e: e.tensor_tensor(out=btmp[:], in0=bre[:], in1=cim_b, op=ALU.mult), reads=["prm", "bre", "bbr"], writes=["btmp"])
    P.op("vector", lambda e: e.tensor_tensor(out=bbi[:], in0=bbi[:], in1=btmp[:], op=ALU.add), reads=["bbi", "btmp"], writes=["bbi"])
    Z = P.sb([128, G2, 128], BF16, "s_Z")
    BT = [P.sb([128, G2, 128], BF16, "s_BTre"), P.sb([128, G2, 128], BF16, "s_BTim")]
    for ri, bb, bkey in ((0, bbr, "bbr"), (1, bbi, "bbi")):
        P.op("gpsimd", lambda e: e.memset(Z[:], 0.0), writes=["Z"])
        Zv = Z[:].rearrange("q (a b) c -> q a b c", b=4)
        bv = bb[:].rearrange("q (a b) h -> q a b h", b=4)
        for g2 in range(2):
            for b in range(4):
                c0 = b * 32 + g2 * 16
                P.op("vector", lambda e, g2=g2, b=b, c0=c0, Zv=Zv, bv=bv: e.tensor_copy(
                    out=Zv[g2 * 64:(g2 + 1) * 64, :, b, c0:c0 + 16], in_=bv[g2 * 64:(g2 + 1) * 64, :, b, :]),
                    reads=[bkey, "Z"], writes=["Z"])
        for gp in range(G2):
            if gp % 8 == 0:
                pT, ptk = C.pT.next()
            P.op("tensor", lambda e, gp=gp, pT=pT: e.transpose(out=pT[:, gp % 8, :], in_=Z[:, gp, :], identity=C.ident[:]),
                 reads=["Z", "ident"], writes=[ptk])
            if gp % 8 == 7:
                C.evac(BT[ri][:, gp - 7:gp + 1, :], pT[:], [ptk], [("BT", ri)])
    Cn = P.sb([128, 8, 64], F32, "s_Cn")
    mask8 = P.sb([128, 8], F32, "s_mask8")
    mask8n = P.sb([128, 8], F32, "s_mask8n")
    P.op("gpsimd", lambda e: e.memset(mask8[:], 1.0), writes=["mask8"])
    P.op("gpsimd", lambda e: e.affine_select(out=mask8[:], in_=mask8[:], pattern=[[-16, 8]], compare_op=ALU.is_ge, fill=0.0,
                                             base=0, channel_multiplier=1), reads=["mask8"], writes=["mask8"])
    P.op("gpsimd", lambda e: e.affine_select(out=mask8[:], in_=mask8[:], pattern=[[16, 8]], compare_op=ALU.is_ge, fill=0.0,
                                             base=15, channel_multiplier=-1), reads=["mask8"], writes=["mask8"])
    P.op("vector", lambda e: e.tensor_scalar(out=mask8n[:], in0=mask8[:], scalar1=-1.0, scalar2=None, op0=ALU.mult), reads=["mask8"], writes=["mask8n"])
    CT = [P.sb([128, G2, 128], BF16, "s_CTre"), P.sb([128, G2, 128], BF16, "s_CTnim")]
    for ri, nm, mk, mkey in ((0, "s5_c_re", mask8, "mask8"), (1, "s5_c_im", mask8n, "mask8n")):
        P.dma("sync", Cn[:], W[nm][li].rearrange("(a r) h p -> (r h) a p", r=8), writes=["Cn"], semkey="s_Cn")
        P.op("vector", lambda e, mk=mk: e.tensor_tensor(
            out=Z[:].rearrange("q (a b) (g p) -> q a (b g) p", b=4, p=64),
            in0=Cn[:].unsqueeze(2).broadcast_to([128, 8, 8, 64]),
            in1=mk[:].unsqueeze(1).unsqueeze(3).broadcast_to([128, 8, 8, 64]), op=ALU.mult),
            reads=["Cn", mkey, "Z"], writes=["Z"])
        for gp in range(G2):
            if gp % 8 == 0:
                pT, ptk = C.pT.next()
            P.op("tensor", lambda e, gp=gp, pT=pT: e.transpose(out=pT[:, gp % 8, :], in_=Z[:, gp, :], identity=C.ident[:]),
                 reads=["Z", "ident"], writes=[ptk])
            if gp % 8 == 7:
                C.evac(CT[ri][:, gp - 7:gp + 1, :], pT[:], [ptk], [("CT", ri)])
    d8 = P.sb([8, 128], F32, "s_d8")
    dcol = P.sb([128, 8], F32, "s_dcol")
    P.dma("sync", d8[:], W["s5_d"][li].rearrange("(c p) -> c p", p=128), writes=["d8"], semkey="s_d8")
    psd, pkd = C.pA.next()
    P.op("tensor", lambda e: e.transpose(out=psd[:, 0:8], in_=d8[:], identity=C.identf[0:8, 0:8]), reads=["d8", "identf"], writes=[pkd])
    P.op("vector", lambda e: e.tensor_copy(out=dcol[:], in_=psd[:, 0:8]), reads=[pkd], writes=["dcol"])
    iot_i = P.sb([128, TBK], mybir.dt.int32, "s_ioti")
    iot = P.sb([128, TBK], F32, "s_iot")
    P.op("gpsimd", lambda e: e.iota(out=iot_i[:], pattern=[[1, TBK]], base=0, channel_multiplier=0), writes=["ioti"])
    P.op("vector", lambda e: e.tensor_copy(out=iot[:], in_=iot_i[:]), reads=["ioti"], writes=["iot"])
    tabr = Rot([(P.sb([128, 2, TBK], F32, f"s_tab{i}"), f"s_tab{i}") for i in range(2)])
    ph = P.sb([128, TBK], F32, "s_ph")
    pki2 = P.sb([128, TBK], mybir.dt.int32, "s_pki2")
    phf = P.sb([128, TBK], F32, "s_phf")
    tq = [P.sb([128, TBK], F32, f"s_tq{i}") for i in range(4)]
    wr = Rot([(P.sb([128, 2, TBK], F32, f"s_w{i}"), f"s_w{i}") for i in range(2)])
    dq = [P.sb([128, TBK], BF16, f"s_dq{i}") for i in range(4)]
    yacc = P.sb([128, L], F32, "s_yacc")
    gt1 = P.sb([128, TBK], F32, "s_gt1")
    gt2 = P.sb([128, TBK], F32, "s_gt2")
    psY = P.ps([128, 2, 512], F32, "s_psY")
    NB5 = TBK // 512
    for kc in range(8):
        for j in range(4):
            gp = kc * 4 + j
            wprev = None
            for b in range(NBK):
                tab, tk = tabr.next()
                if first:
                    P.op("vector", lambda e, gp=gp, b=b: e.tensor_scalar(out=ph[:], in0=iot[:], scalar1=prm[:, 5, gp:gp + 1],
                                                                        scalar2=thb[:, b, gp:gp + 1], op0=ALU.mult, op1=ALU.add),
                         reads=["iot", "prm", "thb"], writes=["ph"])
                    range_reduce(P, ph[:], pki2[:], phf[:], ph[:], ["ph"], ["ph"], "s_rr1")
                    sincos(P, ph[:], tab[:, 0, :], tab[:, 1, :], ["ph"], [(tk, 0)], [(tk, 1)])
                    P.dma("sync", tabs[gp, b], tab[:], reads=[(tk, 0), (tk, 1)], writes=[("tabs", gp, b)], semkey=tk)
                else:
                    P.dma("sync", tab[:], tabs[gp, b], reads=[("tabs", gp, b)], writes=[(tk, 0), (tk, 1)], semkey=tk)
                sT, cT = tab[:, 0, :], tab[:, 1, :]
                tkeys = [(tk, 0), (tk, 1)]
                for hb in range(NB5):
                    t0 = b * TBK + hb * 512
                    sl = slice(hb * 512, (hb + 1) * 512)
                    pre, pkre = C.pA.next()
                    pim, pkim = C.pA.next()
                    xkk = [("xnT", t0 // 128 + q) for q in range(4)]
                    P.op("tensor", lambda e, pre=pre, gp=gp, kc=kc, t0=t0: e.matmul(out=pre[:], lhsT=BT[0][:, gp, :], rhs=xnT[:, kc, t0:t0 + 512],
                                                                                 start=True, stop=True), reads=[("BT", 0)] + xkk, writes=[pkre])
                    P.op("tensor", lambda e, pim=pim, gp=gp, kc=kc, t0=t0: e.matmul(out=pim[:], lhsT=BT[1][:, gp, :], rhs=xnT[:, kc, t0:t0 + 512],
                                                                                 start=True, stop=True), reads=[("BT", 1)] + xkk, writes=[pkim])
                    P.op("vector", lambda e, pre=pre, sl=sl, cT=cT: e.tensor_tensor(out=tq[0][:, sl], in0=cT[:, sl], in1=pre[:], op=ALU.mult),
                         reads=tkeys + [pkre], writes=[("tq0", hb)])
                    P.op("vector", lambda e, pim=pim, sl=sl, sT=sT: e.tensor_tensor(out=tq[1][:, sl], in0=sT[:, sl], in1=pim[:], op=ALU.mult),
                         reads=tkeys + [pkim], writes=[("tq1", hb)])
                    P.op("vector", lambda e, pim=pim, sl=sl, cT=cT: e.tensor_tensor(out=tq[2][:, sl], in0=cT[:, sl], in1=pim[:], op=ALU.mult),
                         reads=tkeys + [pkim], writes=[("tq2", hb)])
                    P.op("vector", lambda e, pre=pre, sl=sl, sT=sT: e.tensor_tensor(out=tq[3][:, sl], in0=sT[:, sl], in1=pre[:], op=ALU.mult),
                         reads=tkeys + [pkre], writes=[("tq3", hb)])
                hbk = lambda i: [(f"tq{i}", hb) for hb in range(NB5)]
                P.op("vector", lambda e: e.tensor_tensor(out=tq[0][:], in0=tq[0][:], in1=tq[1][:], op=ALU.add), reads=hbk(0) + hbk(1), writes=hbk(0))
                P.op("vector", lambda e: e.tensor_tensor(out=tq[2][:], in0=tq[2][:], in1=tq[3][:], op=ALU.subtract), reads=hbk(2) + hbk(3), writes=hbk(2))
                w, wk = wr.next()
                rbc = prm[:, 4, gp:gp + 1].broadcast_to([128, TBK])
                for ci, src_t in ((0, 0), (1, 2)):
                    init = 0.0 if wprev is None else wprev[0][:, ci, TBK - 1:TBK]
                    rdk = [] if wprev is None else [(wprev[1], ci)]
                    P.op("vector", lambda e, w=w, ci=ci, src_t=src_t, init=init, rbc=rbc: e.tensor_tensor_scan(
                        out=w[:, ci, :], data0=rbc, data1=tq[src_t][:], initial=init, op0=ALU.mult, op1=ALU.add),
                        reads=hbk(src_t) + ["prm"] + rdk, writes=[(wk, ci)])
                wprev = (w, wk)
                P.op("vector", lambda e, w=w, cT=cT: e.tensor_tensor(out=dq[0][:], in0=cT, in1=w[:, 0, :], op=ALU.mult),
                     reads=tkeys + [(wk, 0)], writes=["dq0"])
                P.op("vector", lambda e, w=w, sT=sT: e.scalar_tensor_tensor(out=dq[1][:], in0=sT, scalar=-1.0, in1=w[:, 1, :], op0=ALU.mult, op1=ALU.mult),
                     reads=tkeys + [(wk, 1)], writes=["dq1"])
                P.op("vector", lambda e, w=w, sT=sT: e.tensor_tensor(out=dq[2][:], in0=sT, in1=w[:, 0, :], op=ALU.mult),
                     reads=tkeys + [(wk, 0)], writes=["dq2"])
                P.op("vector", lambda e, w=w, cT=cT: e.tensor_tensor(out=dq[3][:], in0=cT, in1=w[:, 1, :], op=ALU.mult),
                     reads=tkeys + [(wk, 1)], writes=["dq3"])
                for hb in range(NB5):
                    sl = slice(hb * 512, (hb + 1) * 512)
                    t0 = b * TBK + hb * 512
                    for qi in range(4):
                        P.op("tensor", lambda e, hb=hb, qi=qi, sl=sl, gp=gp: e.matmul(out=psY[:, hb, :], lhsT=CT[0 if qi < 2 else 1][:, gp, :],
                                                                                   rhs=dq[qi][:, sl], start=(qi == 0), stop=(qi == 3)),
                             reads=[("CT", 0), ("CT", 1), f"dq{qi}"], writes=[("psY", hb)])
                    if j == 0:
                        P.op("vector", lambda e, hb=hb, t0=t0: e.tensor_copy(out=yacc[:, t0:t0 + 512], in_=psY[:, hb, :]),
                             reads=[("psY", hb)], writes=[("yacc", t0 // 512)])
                    else:
                        P.op("vector", lambda e, hb=hb, t0=t0: e.tensor_tensor(out=yacc[:, t0:t0 + 512], in0=yacc[:, t0:t0 + 512], in1=psY[:, hb, :], op=ALU.add),
                             reads=[("psY", hb), ("yacc", t0 // 512)], writes=[("yacc", t0 // 512)])
        for b in range(NBK):
            sl = slice(b * TBK, (b + 1) * TBK)
            yk = [("yacc", b * NB5 + q) for q in range(NB5)]
            xk = [("xnT", b * (TBK // 128) + q) for q in range(TBK // 128)]
            P.op("vector", lambda e, sl=sl, kc=kc: e.scalar_tensor_tensor(out=yacc[:, sl], in0=xnT[:, kc, sl], scalar=dcol[:, kc:kc + 1], in1=yacc[:, sl],
                                                                        op0=ALU.mult, op1=ALU.add), reads=yk + xk + ["dcol"], writes=yk)
            P.op("vector", lambda e, sl=sl: e.tensor_tensor(out=gt1[:], in0=yacc[:, sl], in1=yacc[:, sl], op=ALU.mult), reads=yk, writes=["gt1"])
            P.op("vector", lambda e: e.tensor_scalar(out=gt1[:], in0=gt1[:], scalar1=0.044715, scalar2=1.0, op0=ALU.mult, op1=ALU.add), reads=["gt1"], writes=["gt1"])
            P.op("vector", lambda e, sl=sl: e.tensor_tensor(out=gt2[:], in0=gt1[:], in1=yacc[:, sl], op=ALU.mult), reads=yk + ["gt1"], writes=["gt2"])
            P.op("scalar", lambda e: e.activation(out=gt2[:], in_=gt2[:], func=AF.Sigmoid, scale=1.5957691216057308), reads=["gt2"], writes=["gt2"])
            P.op("vector", lambda e, sl=sl, kc=kc: e.tensor_tensor(out=xnT[:, kc, sl], in0=yacc[:, sl], in1=gt2[:], op=ALU.mult),
                 reads=yk + ["gt2"], writes=xk)
    P.pop()
    P.push()
    wgl = P.sb([128, 8, 2 * D], BF16, "s_wgl")
    wv = W["s5_w_glu"][li].rearrange("(c p) n -> p c n", p=128)
    for k in range(4):
        P.dma("gpsimd", wgl[:, :, k * 512:(k + 1) * 512], wv[:, :, k * 512:(k + 1) * 512], writes=["wgl"], semkey="s_wgl")
    bgl = P.sb([1, 2 * D], BF16, "s_bgl")
    ones1 = P.sb([1, 128], BF16, "s_ones1")
    P.dma("gpsimd", bgl[:], W["s5_b_glu"][li:li + 1, :], writes=["bgl"], semkey="s_bgl")
    P.op("gpsimd", lambda e: e.memset(ones1[:], 1.0), writes=["ones1"])
    sgr = Rot([(P.sb([128, D], F32, f"s_sg{i}"), f"s_sg{i}") for i in range(2)])
    for n in range(NT):
        tile = tile0 + n
        sg, sgk = sgr.next()
        for k in (2, 3):
            ps, pk = C.pA.next()
            for c in range(8):
                P.op("tensor", lambda e, c=c, ps=ps, n=n, k=k: e.matmul(out=ps[:], lhsT=xnT[:, c, n * 128:(n + 1) * 128],
                                                                    rhs=wgl[:, c, k * 512:(k + 1) * 512], start=(c == 0), stop=False),
                     reads=["wgl", ("xnT", n)], writes=[pk])
            P.op("tensor", lambda e, ps=ps, k=k: e.matmul(out=ps[:], lhsT=ones1[0:1, :], rhs=bgl[0:1, k * 512:(k + 1) * 512], start=False, stop=True),
                 reads=["bgl", "ones1"], writes=[pk])
            P.op("scalar", lambda e, ps=ps, sg=sg, k=k: e.activation(out=sg[:, (k - 2) * 512:(k - 1) * 512], in_=ps[:], func=AF.Sigmoid),
                 reads=[pk], writes=[(sgk, k)])
        ht, hk = C.ht.next()
        P.dma("sync", ht[:], hsrc[tile * 128:(tile + 1) * 128, :], reads=hkeys(tile), writes=[hk], semkey=hk)
        for k in (0, 1):
            ps, pk = C.pA.next()
            for c in range(8):
                P.op("tensor", lambda e, c=c, ps=ps, n=n, k=k: e.matmul(out=ps[:], lhsT=xnT[:, c, n * 128:(n + 1) * 128],
                                                                    rhs=wgl[:, c, k * 512:(k + 1) * 512], start=(c == 0), stop=False),
                     reads=["wgl", ("xnT", n)], writes=[pk])
            P.op("tensor", lambda e, ps=ps, k=k: e.matmul(out=ps[:], lhsT=ones1[0:1, :], rhs=bgl[0:1, k * 512:(k + 1) * 512], start=False, stop=True),
                 reads=["bgl", "ones1"], writes=[pk])
            P.op("vector", lambda e, ps=ps, sg=sg, k=k: e.tensor_tensor(out=sg[:, k * 512:(k + 1) * 512], in0=sg[:, k * 512:(k + 1) * 512], in1=ps[:], op=ALU.mult),
                 reads=[pk, (sgk, k + 2)], writes=[(sgk, k + 2)])
            P.op("vector", lambda e, sg=sg, ht=ht, k=k: e.tensor_tensor(out=ht[:, k * 512:(k + 1) * 512], in0=ht[:, k * 512:(k + 1) * 512],
                                                                      in1=sg[:, k * 512:(k + 1) * 512], op=ALU.add),
                 reads=[(sgk, k + 2), hk], writes=[hk])
        P.dma("sync", hdst[tile * 128:(tile + 1) * 128, :], ht[:], reads=[hk], writes=hkeys(tile), semkey=hk)
    P.pop()
    P.pop()


RW_STOP = 0


def stage_rw(C, hsrc, hdst, tile0, L, W, li, ysc, vsc):
    P = C.P
    NT = L // 128
    HB = min(L, 1024)
    NHB = L // HB
    TB4 = 4
    w_in = W["rw_w_in"][li]
    wv_ = w_in.rearrange("(c p) n -> p c n", p=128)
    P.push()
    xnT = P.sb([128, 8, L], BF16, "r_xnT")
    dxT = P.sb([128, 8, L], BF16, "r_dxT")
    load_gain(C, W["rw_norm_g"][li:li + 1, :])
    norm_T(C, hsrc, tile0, NT, xnT, "xnT")
    xall = [("xnT", n) for n in range(NT)]
    for c in range(8):
        eng = "vector"
        P.op(eng, lambda e, c=c: e.tensor_tensor(out=dxT[:, c, 1:L], in0=xnT[:, c, 0:L - 1], in1=xnT[:, c, 1:L], op=ALU.subtract),
             reads=xall, writes=[("dxT", c)])
        P.op(eng, lambda e, c=c: e.tensor_scalar(out=dxT[:, c, 0:1], in0=xnT[:, c, 0:1], scalar1=-1.0, scalar2=None, op0=ALU.mult),
             reads=xall + [("dxT", c)], writes=[("dxT", c)])
    dall = [("dxT", c) for c in range(8)]
    prow = P.sb([48, 128], F32, "r_prow")
    P.dma("sync", prow[:], W["rw_mu"][li].rearrange("i (c p) -> (i c) p", p=128), writes=["prow"], semkey="r_prow")
    psm, pkm = C.pA.next()
    P.op("tensor", lambda e: e.transpose(out=psm[:, 0:48], in_=prow[:], identity=C.identf[0:48, 0:48]), reads=["prow", "identf"], writes=[pkm])
    mucol = P.sb([128, 6, 8], F32, "r_mucol")
    P.op("vector", lambda e: e.tensor_copy(out=mucol[:].rearrange("p i c -> p (i c)"), in_=psm[:, 0:48]), reads=[pkm], writes=["mucol"])
    prow2 = P.sb([40, 128], F32, "r_prow2")
    for k, nm in enumerate(("rw_w0", "rw_a0", "rw_k_k", "rw_k_a", "rw_r_k")):
        P.dma("sync", prow2[k * 8:(k + 1) * 8, :], W[nm][li].rearrange("(c p) -> c p", p=128), writes=[("prow2", k)], semkey=f"r_prow2{k}")
    psm2, pkm2 = C.pA.next()
    P.op("tensor", lambda e: e.transpose(out=psm2[:, 0:40], in_=prow2[:], identity=C.identf[0:40, 0:40]),
         reads=[("prow2", k) for k in range(5)] + ["identf"], writes=[pkm2])
    pcol = P.sb([128, 6, 8], F32, "r_pcol")
    P.op("vector", lambda e: e.tensor_copy(out=pcol[:, 0:5, :].rearrange("p i c -> p (i c)"), in_=psm2[:, 0:40]), reads=[pkm2], writes=["pcol"])
    P.op("vector", lambda e: e.tensor_scalar(out=pcol[:, 5, :], in0=pcol[:, 3, :], scalar1=-1.0, scalar2=1.0, op0=ALU.mult, op1=ALU.add),
         reads=["pcol"], writes=["pcol"])
    maskS = P.sb([128, 128], BF16, "r_maskS")
    maskST = P.sb([128, 128], BF16, "r_maskST")
    mtmp = P.sb([128, 128], F32, "r_mtmp")
    P.op("gpsimd", lambda e: e.affine_select(out=mtmp[:], in_=C.onesf[:], pattern=[[1, 128]], compare_op=ALU.is_ge, fill=0.0, base=-1,
                                             channel_multiplier=-1), reads=["onesf"], writes=["mtmp"])
    P.op("vector", lambda e: e.tensor_copy(out=maskS[:], in_=mtmp[:]), reads=["mtmp"], writes=["maskS"])
    P.op("gpsimd", lambda e: e.affine_select(out=mtmp[:], in_=C.onesf[:], pattern=[[-1, 128]], compare_op=ALU.is_ge, fill=0.0, base=-1,
                                             channel_multiplier=1), reads=["onesf", "maskS"], writes=["mtmp"])
    P.op("vector", lambda e: e.tensor_copy(out=maskST[:], in_=mtmp[:]), reads=["mtmp"], writes=["maskST"])
    Gt = P.sb([8, 3, 128], F32, "r_Gt")
    mk16 = P.sb([128, 128], F32, "r_mk16")
    d32 = P.sb([128, 128], F32, "r_d32")
    d64 = P.sb([128, 128], F32, "r_d64")
    d128 = P.sb([128, 128], F32, "r_d128")
    P.op("gpsimd", lambda e: e.memset(Gt[:], 1.0), writes=["Gt"])
    for gi, (bsz, nbk) in enumerate(((16, 8), (32, 4), (64, 2))):
        P.op("gpsimd", lambda e, gi=gi, bsz=bsz, nbk=nbk: e.affine_select(out=Gt[0:nbk, gi, :], in_=Gt[0:nbk, gi, :], pattern=[[1, 128]],
                                                                         compare_op=ALU.is_ge, fill=0.0, base=0, channel_multiplier=-bsz),
             reads=["Gt"], writes=["Gt"])
        P.op("gpsimd", lambda e, gi=gi, bsz=bsz, nbk=nbk: e.affine_select(out=Gt[0:nbk, gi, :], in_=Gt[0:nbk, gi, :], pattern=[[-1, 128]],
                                                                         compare_op=ALU.is_ge, fill=0.0, base=bsz - 1, channel_multiplier=bsz),
             reads=["Gt"], writes=["Gt"])
    psG, pkG = C.pA.next()
    for gi, nbk in enumerate((8, 4, 2)):
        P.op("tensor", lambda e, gi=gi, nbk=nbk: e.matmul(out=psG[:, gi * 128:(gi + 1) * 128], lhsT=Gt[0:nbk, gi, :], rhs=Gt[0:nbk, gi, :],
                                                          start=True, stop=True), reads=["Gt"], writes=[pkG])
    P.op("vector", lambda e: e.tensor_copy(out=mk16[:], in_=psG[:, 0:128]), reads=[pkG], writes=["mk16"])
    P.op("vector", lambda e: e.tensor_copy(out=mtmp[:], in_=psG[:, 128:256]), reads=[pkG, "maskST"], writes=["mtmp"])
    P.op("vector", lambda e: e.tensor_tensor(out=d32[:], in0=mtmp[:], in1=mk16[:], op=ALU.subtract), reads=["mtmp", "mk16"], writes=["d32"])
    P.op("vector", lambda e: e.tensor_tensor(out=d64[:], in0=psG[:, 256:384], in1=mtmp[:], op=ALU.subtract), reads=[pkG, "mtmp"], writes=["d64"])
    P.op("vector", lambda e: e.tensor_scalar(out=d128[:], in0=psG[:, 256:384], scalar1=-1.0, scalar2=1.0, op0=ALU.mult, op1=ALU.add),
         reads=[pkG], writes=["d128"])
    bones = P.sb([128, 128], BF16, "r_bones")
    bsel = P.sb([128, 8], BF16, "r_bsel")
    P.op("gpsimd", lambda e: e.memset(bones[:], 0.0), writes=["bones"])
    P.op("gpsimd", lambda e: e.memset(bones[0:64, 0:64], 1.0), reads=["bones"], writes=["bones"])
    P.op("gpsimd", lambda e: e.memset(bones[64:128, 64:128], 1.0), reads=["bones"], writes=["bones"])
    P.op("gpsimd", lambda e: e.memset(bsel[:], 0.0), writes=["bsel"])
    P.op("gpsimd", lambda e: e.memset(bsel[0:64, 0:1], 1.0), reads=["bsel"], writes=["bsel"])
    P.op("gpsimd", lambda e: e.memset(bsel[64:128, 1:2], 1.0), reads=["bsel"], writes=["bsel"])
    onec = P.sb([128, 1], F32, "r_onec")
    P.op("gpsimd", lambda e: e.memset(onec[:], 1.0), writes=["onec"])
    bsc = P.sb([128, NT, 16], F32, "r_bsc")

    def proj2(ps, wt, wts, wkeys, cols, tok, tokmajor):
        for kc in range(8):
            for which, (src_t, ww) in enumerate(((xnT, wt), (dxT, wts))):
                first = (kc == 0 and which == 0)
                last = (kc == 7 and which == 1)
                if tokmajor:
                    P.op("tensor", lambda e, kc=kc, src_t=src_t, ww=ww, first=first, last=last: e.matmul(
                        out=ps, lhsT=src_t[:, kc, tok], rhs=ww[:, kc, cols], start=first, stop=last),
                        reads=wkeys + xall + dall, writes=[ps_key[0]])
                else:
                    P.op("tensor", lambda e, kc=kc, src_t=src_t, ww=ww, first=first, last=last: e.matmul(
                        out=ps, lhsT=ww[:, kc, cols], rhs=src_t[:, kc, tok], start=first, stop=last),
                        reads=wkeys + xall + dall, writes=[ps_key[0]])
    ps_key = [None]
    twT = P.sb([64, L], BF16, "r_twT")
    taT = P.sb([64, L], BF16, "r_taT")
    tg1 = P.sb([128, L], BF16, "r_tg1")
    tg2 = P.sb([32, L], BF16, "r_tg2")
    P.push()
    wl = P.sb([128, 8, 288], BF16, "r_wl")
    wls = P.sb([128, 8, 288], BF16, "r_wls")
    P.dma("gpsimd", wl[:], wv_[:, :, 3072:3360], writes=["wl"], semkey="r_wl")
    for (c0, c1, mi) in ((0, 64, 1), (64, 128, 4), (128, 288, 5)):
        P.op("vector", lambda e, c0=c0, c1=c1, mi=mi: e.tensor_tensor(out=wls[:, :, c0:c1], in0=wl[:, :, c0:c1],
                                                                    in1=mucol[:, mi, :].unsqueeze(2).broadcast_to([128, 8, c1 - c0]), op=ALU.mult),
             reads=["wl", "mucol"], writes=["wls"])
    for tb in range(L // 512):
        tok = slice(tb * 512, (tb + 1) * 512)
        for (c0, c1, dst, fn, dk) in ((0, 64, twT, AF.Tanh, "twT"), (64, 128, taT, AF.Copy, "taT"), (128, 256, tg1, AF.Sigmoid, "tg1"),
                                      (256, 288, tg2, AF.Sigmoid, "tg2")):
            ps, pk = C.pA.next()
            ps_key[0] = pk
            proj2(ps[0:c1 - c0, :], wl, wls, ["wl", "wls"], slice(c0, c1), tok, False)
            if fn == AF.Copy:
                C.evac(dst[:, tok], ps[0:c1 - c0, :], [pk], [(dk, tb)])
            else:
                P.op("scalar", lambda e, ps=ps, dst=dst, fn=fn, tok=tok, c0=c0, c1=c1: e.activation(out=dst[:, tok], in_=ps[0:c1 - c0, :], func=fn),
                     reads=[pk], writes=[(dk, tb)])
    lkeys = lambda nm: [(nm, tb) for tb in range(L // 512)]
    P.pop()
    P.push()
    if RW_STOP == 1:
        P.pop(); P.pop()
        return
    wup_w = P.sb([64, D], BF16, "r_wupw")
    wup_a = P.sb([64, D], BF16, "r_wupa")
    P.dma("gpsimd", wup_w[:], W["rw_w_up"][li], writes=["wupw"], semkey="r_wupw")
    P.dma("gpsimd", wup_a[:], W["rw_a_up"][li], writes=["wupa"], semkey="r_wupa")
    w3r = Rot([(P.sb([128, 8, 3, 128], BF16, f"r_w3{i}"), f"r_w3{i}") for i in range(1)])
    w3s = P.sb([128, 8, 3, 128], BF16, "r_w3s")
    fA, fB, fC, fD, fE, fF, fG = [P.sb([128, HB], F32, f"r_f{i}") for i in range(7)]
    rT = P.sb([128, L], BF16, "r_rT")
    aT = P.sb([128, L], BF16, "r_aT")
    kT = P.sb([128, L], BF16, "r_kT")
    bT = P.sb([128, L], BF16, "r_bT")
    rkT = P.sb([128, L], BF16, "r_rkT")
    sqb = P.sb([128, HB], BF16, "r_sqb")
    gam = P.sb([128, NT], F32, "r_gam")
    cst = P.sb([128, NT], F32, "r_cst")
    ktok = P.sb([128, NT, 128], BF16, "r_ktok")
    btok = P.sb([128, NT, 128], BF16, "r_btok")
    vtok = P.sb([128, NT, 128], BF16, "r_vtok")
    NU = TB4 * 2
    Mst = {nm: P.sb([128, NU, 128], BF16, "r_M" + nm) for nm in ("N", "NT", "M2", "M3", "M4", "R", "P2", "PT2")}
    STf = P.sb([128, 128], F32, "r_STf")
    STb = P.sb([128, 128], BF16, "r_STb")
    PTb = Rot([(P.sb([128, 128], BF16, f"r_PTb{i}"), f"r_PTb{i}") for i in range(2)])
    UTb = Rot([(P.sb([128, 128], BF16, f"r_UTb{i}"), f"r_UTb{i}") for i in range(2)])
    ytr = Rot([(P.sb([128, 128], F32, f"r_yt{i}"), f"r_yt{i}") for i in range(2)])
    psX = P.ps([128, 2, 512], F32, "r_psX")
    psXi = [0]

    def nextX():
        s = psXi[0] % 2
        psXi[0] += 1
        return psX[:, s, :], ("psX", s)
    for c in range(8):
        cols = slice(0, 128)
        w3, w3k = w3r.next()
        for i3, base in enumerate((0, 1024, 2048)):
            P.dma("gpsimd", w3[:, :, i3, :], wv_[:, :, base + c * 128: base + (c + 1) * 128], writes=[(w3k, i3)], semkey=w3k)
        for i3, mi in enumerate((0, 2, 3)):
            P.op("vector", lambda e, i3=i3, mi=mi, w3=w3: e.tensor_tensor(out=w3s[:, :, i3, :], in0=w3[:, :, i3, :],
                                                                       in1=mucol[:, mi, :].unsqueeze(2).broadcast_to([128, 8, 128]), op=ALU.mult),
                 reads=[(w3k, i3), "mucol"], writes=[("w3s", i3)])
        for n in range(NT):
            ps, pk = C.pA.next()
            ps_key[0] = pk
            proj2(ps[:, 0:128], w3[:, :, 2, :], w3s[:, :, 2, :], [(w3k, 2), ("w3s", 2)], cols, slice(n * 128, (n + 1) * 128), True)
            C.evac(vtok[:, n, :], ps[:, 0:128], [pk], [("vtok", n)])
        tile_ = None
        P.dma("sync", vsc[tile0 * 128:(tile0 + NT) * 128, c * 128:(c + 1) * 128].rearrange("(n p) d -> p n d", p=128), vtok[:],
              reads=[("vtok", n) for n in range(NT)], writes=[("vsc", c)], semkey="r_vtok")
        if RW_STOP == 1.2:
            P.pop(); P.pop()
            return
        for hbk in range(NHB):
            hs = slice(hbk * HB, (hbk + 1) * HB)
            n0 = hbk * (HB // 128)
            nn = HB // 128
            fk = lambda nm: [(nm, hbk)]
            for q in range(HB // 512):
                tok = slice(hbk * HB + q * 512, hbk * HB + (q + 1) * 512)
                ql = slice(q * 512, (q + 1) * 512)
                tbi = (hbk * HB) // 512 + q
                ps, pk = C.pA.next(); ps_key[0] = pk
                proj2(ps[:], w3[:, :, 0, :], w3s[:, :, 0, :], [(w3k, 0), ("w3s", 0)], cols, tok, False)
                C.evac(fA[:, ql], ps[:], [pk], [("fA", q)])
                ps, pk = C.pA.next(); ps_key[0] = pk
                proj2(ps[:], w3[:, :, 1, :], w3s[:, :, 1, :], [(w3k, 1), ("w3s", 1)], cols, tok, False)
                C.evac(fB[:, ql], ps[:], [pk], [("fB", q)])
                ps, pk = C.pA.next()
                P.op("tensor", lambda e, ps=ps, tok=tok, c=c: e.matmul(out=ps[:], lhsT=wup_a[:, c * 128:(c + 1) * 128], rhs=taT[:, tok], start=True, stop=True),
                     reads=["wupa", ("taT", tbi)], writes=[pk])
                P.op("scalar", lambda e, ps=ps, ql=ql, c=c: e.activation(out=fC[:, ql], in_=ps[:], func=AF.Sigmoid, bias=pcol[:, 1, c:c + 1]),
                     reads=[pk, "pcol"], writes=[("fC", q)])
                ps, pk = C.pA.next()
                P.op("tensor", lambda e, ps=ps, tok=tok, c=c: e.matmul(out=ps[:], lhsT=wup_w[:, c * 128:(c + 1) * 128], rhs=twT[:, tok], start=True, stop=True),
                     reads=["wupw", ("twT", tbi)], writes=[pk])
                P.op("scalar", lambda e, ps=ps, ql=ql, c=c: e.activation(out=fD[:, ql], in_=ps[:], func=AF.Sigmoid, bias=pcol[:, 0, c:c + 1]),
                     reads=[pk, "pcol"], writes=[("fD", q)])
            if RW_STOP == 1.4:
                P.pop(); P.pop()
                return
            qk = lambda nm: [(nm, q) for q in range(HB // 512)]
            NEG = -0.6065306597126334
            P.op("vector", lambda e: e.tensor_scalar(out=fG[:], in0=fD[:], scalar1=NEG, scalar2=None, op0=ALU.mult), reads=qk("fD"), writes=["fG"])
            P.op("vector", lambda e: e.tensor_tensor_scan(out=fD[:], data0=onec[:, 0:1].broadcast_to([128, HB]), data1=fG[:], initial=0.0,
                                                          op0=ALU.mult, op1=ALU.add), reads=["fG", "onec"] + qk("fD"), writes=qk("fD"))
            fD3 = fD[:].rearrange("p (n t) -> p n t", t=128)
            P.op("vector", lambda e, n0=n0: e.memset(cst[:, n0:n0 + 1], 0.0), writes=[("cst", hbk)])
            if nn > 1:
                P.op("vector", lambda e, n0=n0, fD3=fD3: e.tensor_copy(out=cst[:, n0 + 1:n0 + nn], in_=fD3[:, 0:nn - 1, 127]),
                     reads=qk("fD") + [("cst", hbk)], writes=[("cst", hbk)])
            P.op("vector", lambda e, n0=n0, fD3=fD3: e.tensor_tensor(out=fD3, in0=fD3, in1=cst[:, n0:n0 + nn].unsqueeze(2).broadcast_to([128, nn, 128]),
                                                                   op=ALU.subtract), reads=qk("fD") + [("cst", hbk)], writes=qk("fD"))
            if RW_STOP == 1.6:
                P.pop(); P.pop()
                return
            P.op("vector", lambda e, c=c: e.tensor_scalar(out=fE[:], in0=fB[:], scalar1=pcol[:, 2, c:c + 1], scalar2=None, op0=ALU.mult),
                 reads=qk("fB") + ["pcol"], writes=["fE"])
            P.op("vector", lambda e: e.tensor_tensor(out=sqb[:], in0=fE[:], in1=fE[:], op=ALU.mult), reads=["fE"], writes=["sqb"])
            for q in range(HB // 512):
                ql = slice(q * 512, (q + 1) * 512)
                ps, pk = C.pA.next()
                P.op("tensor", lambda e, ps=ps, ql=ql: e.matmul(out=ps[:], lhsT=bones[:], rhs=sqb[:, ql], start=True, stop=True),
                     reads=["bones", "sqb"], writes=[pk])
                P.op("scalar", lambda e, ps=ps, ql=ql: e.activation(out=fF[:, ql], in_=ps[:], func=AF.Sqrt), reads=[pk], writes=[("fF", q)])
            P.op("vector", lambda e: e.tensor_scalar(out=fF[:], in0=fF[:], scalar1=1e-12, scalar2=None, op0=ALU.max), reads=qk("fF"), writes=qk("fF"))
            P.op("vector", lambda e: e.reciprocal(out=fF[:], in_=fF[:]), reads=qk("fF"), writes=qk("fF"))
            P.op("vector", lambda e: e.tensor_tensor(out=fE[:], in0=fE[:], in1=fF[:], op=ALU.mult), reads=["fE"] + qk("fF"), writes=["fE"])
            if RW_STOP == 1.7:
                P.pop(); P.pop()
                return
            P.op("vector", lambda e, c=c: e.tensor_scalar(out=fF[:], in0=fC[:], scalar1=pcol[:, 3, c:c + 1], scalar2=pcol[:, 5, c:c + 1],
                                                          op0=ALU.mult, op1=ALU.add), reads=qk("fC") + ["pcol"] + qk("fF"), writes=qk("fF"))
            P.op("vector", lambda e: e.tensor_tensor(out=fB[:], in0=fB[:], in1=fF[:], op=ALU.mult), reads=qk("fB") + qk("fF"), writes=qk("fB"))
            P.op("vector", lambda e, c=c, hs=hs: e.scalar_tensor_tensor(out=rkT[:, hs], in0=fA[:], scalar=pcol[:, 4, c:c + 1], in1=fB[:],
                                                                      op0=ALU.mult, op1=ALU.mult), reads=qk("fA") + qk("fB") + ["pcol"], writes=[("rkT", hbk)])
            P.op("scalar", lambda e: e.activation(out=fF[:], in_=fD[:], func=AF.Exp), reads=qk("fD") + qk("fF"), writes=qk("fF"))
            P.op("vector", lambda e, hs=hs: e.tensor_tensor(out=rT[:, hs], in0=fA[:], in1=fF[:], op=ALU.mult), reads=qk("fA") + qk("fF"), writes=[("rT", hbk)])
            fF3 = fF[:].rearrange("p (n t) -> p n t", t=128)
            P.op("vector", lambda e, n0=n0, fF3=fF3: e.tensor_copy(out=gam[:, n0:n0 + nn], in_=fF3[:, :, 127]), reads=qk("fF"), writes=[("gam", hbk)])
            P.op("vector", lambda e: e.tensor_tensor(out=fA[:], in0=fD[:], in1=fG[:], op=ALU.subtract), reads=qk("fD") + ["fG"] + qk("fA") + [("rT", hbk), ("rkT", hbk)],
                 writes=qk("fA"))
            P.op("scalar", lambda e: e.activation(out=fA[:], in_=fA[:], func=AF.Exp), reads=qk("fA"), writes=qk("fA"))
            P.op("vector", lambda e, hs=hs: e.scalar_tensor_tensor(out=aT[:, hs], in0=fE[:], scalar=-1.0, in1=fA[:], op0=ALU.mult, op1=ALU.mult),
                 reads=["fE"] + qk("fA"), writes=[("aT", hbk)])
            P.op("scalar", lambda e: e.activation(out=fF[:], in_=fD[:], func=AF.Exp, scale=-1.0), reads=qk("fD") + qk("fF") + [("rT", hbk), ("gam", hbk)],
                 writes=qk("fF"))
            P.op("vector", lambda e, hs=hs: e.tensor_tensor(out=kT[:, hs], in0=fB[:], in1=fF[:], op=ALU.mult), reads=qk("fB") + qk("fF"), writes=[("kT", hbk)])
            P.op("vector", lambda e: e.tensor_tensor(out=fE[:], in0=fE[:], in1=fC[:], op=ALU.mult), reads=["fE"] + qk("fC") + [("aT", hbk)], writes=["fE"])
            P.op("vector", lambda e, hs=hs: e.tensor_tensor(out=bT[:, hs], in0=fE[:], in1=fF[:], op=ALU.mult), reads=["fE"] + qk("fF"), writes=[("bT", hbk)])
            if RW_STOP == 1.8:
                P.pop(); P.pop()
                return
            for n in range(n0, n0 + nn):
                tsl = slice(n * 128, (n + 1) * 128)
                pT, ptk = C.pT.next()
                P.op("tensor", lambda e, pT=pT, tsl=tsl: e.transpose(out=pT[:, 0, :], in_=kT[:, tsl], identity=C.ident[:]), reads=[("kT", hbk), "ident"], writes=[ptk])
                P.op("tensor", lambda e, pT=pT, tsl=tsl: e.transpose(out=pT[:, 1, :], in_=bT[:, tsl], identity=C.ident[:]), reads=[("bT", hbk), "ident"], writes=[ptk])
                ev = C.copy_eng()
                C.evac(ktok[:, n, :], pT[:, 0, :], [ptk], [("ktok", n)], eng=ev)
                C.evac(btok[:, n, :], pT[:, 1, :], [ptk], [("btok", n)], eng=ev)
                ps, pk = C.pA.next()
                P.op("tensor", lambda e, ps=ps, tsl=tsl: e.matmul(out=ps[:, 0:8], lhsT=rkT[:, tsl], rhs=bsel[:], start=True, stop=True),
                     reads=[("rkT", hbk), "bsel"], writes=[pk])
                P.op("vector", lambda e, ps=ps, n=n, c=c: e.tensor_copy(out=bsc[:, n, 2 * c:2 * c + 2], in_=ps[:, 0:2]), reads=[pk], writes=[("bsc", n)])
        if RW_STOP == 2:
            P.pop(); P.pop()
            return
        P.op("vector", lambda e: e.memset(STf[:], 0.0), writes=["STf"])
        P.op("gpsimd", lambda e: e.memset(STb[:], 0.0), writes=["STb"])
        for n0 in range(0, NT, TB4):
            nb = min(TB4, NT - n0)
            units = [(n, hh) for hh in range(2) for n in range(n0, n0 + nb)]
            uidx = lambda n, hh: hh * nb + (n - n0)
            hb_of = lambda n: [(nm, (n * 128) // HB) for nm in ()]
            def fm(nm, n):
                return (nm, (n * 128) // HB)
            specs = (("N", bT, aT, "bT", "aT", maskS), ("NT", aT, bT, "aT", "bT", maskST), ("M2", kT, aT, "kT", "aT", maskS),
                     ("M3", bT, rT, "bT", "rT", C.mask01), ("M4", kT, rT, "kT", "rT", C.mask01))
            nun = len(units)
            nug = (nun + 3) // 4
            gk = lambda nm: [("M" + nm, g) for g in range(nug)]

            def scores(sp):
                for nm, lt, rt, lk, rkk, msk in sp:
                    for u0 in range(0, nun, 4):
                        ps, pk = C.pA.next()
                        for ui in range(u0, min(u0 + 4, nun)):
                            n, hh = units[ui]
                            p0 = hh * 64
                            tsl = slice(n * 128, (n + 1) * 128)
                            P.op("tensor", lambda e, ps=ps, ui=ui, u0=u0, p0=p0, tsl=tsl, lt=lt, rt=rt: e.matmul(
                                out=ps[:, (ui - u0) * 128:(ui - u0 + 1) * 128], lhsT=lt[p0:p0 + 64, tsl], rhs=rt[p0:p0 + 64, tsl], start=True, stop=True),
                                reads=[fm(lk, n), fm(rkk, n)], writes=[pk])
                        for uu in range(min(4, nun - u0)):
                            P.op("vector", lambda e, ps=ps, nm=nm, u0=u0, uu=uu, msk=msk: e.tensor_tensor(
                                out=Mst[nm][:, u0 + uu, :], in0=ps[:, uu * 128:(uu + 1) * 128], in1=msk[:], op=ALU.mult),
                                reads=[pk, "maskS", "maskST", "mask01"], writes=[("M" + nm, u0 // 4)])

            def mm4(out_name, lname, rname, pool):
                res = []
                for u0 in range(0, nun, 4):
                    nu = min(4, nun - u0)
                    g = u0 // 4
                    ps, pk = pool()
                    for ui in range(u0, u0 + nu):
                        P.op("tensor", lambda e, ps=ps, ui=ui, u0=u0: e.matmul(
                            out=ps[:, (ui - u0) * 128:(ui - u0 + 1) * 128], lhsT=Mst[lname][:, ui, :], rhs=Mst[rname][:, ui, :], start=True, stop=True),
                            reads=[("M" + lname, g), ("M" + rname, g)], writes=[pk])
                    res.append((ps, pk, u0, nu))
                return res

            def evac_copy(res, dst, eng):
                for ps, pk, u0, nu in res:
                    v = ps[:, 0:nu * 128].rearrange("p (u t) -> p u t", t=128)
                    if eng == "vector":
                        P.op("vector", lambda e, v=v, u0=u0, nu=nu: e.tensor_copy(out=Mst[dst][:, u0:u0 + nu, :], in_=v), reads=[pk], writes=[("M" + dst, u0 // 4)])
                    else:
                        P.op("scalar", lambda e, v=v, u0=u0, nu=nu: e.copy(out=Mst[dst][:, u0:u0 + nu, :], in_=v), reads=[pk], writes=[("M" + dst, u0 // 4)])

            def evac_add(res, dst):
                for ps, pk, u0, nu in res:
                    v = ps[:, 0:nu * 128].rearrange("p (u t) -> p u t", t=128)
                    P.op("vector", lambda e, v=v, u0=u0, nu=nu: e.tensor_tensor(out=Mst[dst][:, u0:u0 + nu, :], in0=Mst[dst][:, u0:u0 + nu, :], in1=v, op=ALU.add),
                         reads=[pk, ("M" + dst, u0 // 4)], writes=[("M" + dst, u0 // 4)])

            def masked(dst, srcn, msk, mkey):
                for uu in range(nun):
                    P.op("vector", lambda e, uu=uu: e.tensor_tensor(out=Mst[dst][:, uu, :], in0=Mst[srcn][:, uu, :], in1=msk[:], op=ALU.mult),
                         reads=[("M" + srcn, uu // 4), mkey], writes=[("M" + dst, uu // 4)])
            scores(specs[0:2])
            if RW_STOP == 2.5:
                P.pop(); P.pop()
                return
            masked("P2", "N", mk16, "mk16")
            masked("PT2", "NT", mk16, "mk16")
            for uu in range(nun):
                P.op("vector", lambda e, uu=uu: e.tensor_tensor(out=Mst["R"][:, uu, :], in0=Mst["P2"][:, uu, :], in1=C.identf[:], op=ALU.add),
                     reads=[("MP2", uu // 4), "identf"], writes=[("MR", uu // 4)])
                P.op("vector", lambda e, uu=uu: e.tensor_tensor(out=Mst["M4"][:, uu, :], in0=Mst["PT2"][:, uu, :], in1=C.identf[:], op=ALU.add),
                     reads=[("MPT2", uu // 4), "identf"], writes=[("MM4", uu // 4)])
            cur, curT, nxt, nxtT = "P2", "PT2", "M2", "M3"
            for lev in range(3):
                rA = mm4(nxt, curT, cur, C.pA.next)
                rB = mm4(nxtT, cur, curT, nextX)
                evac_copy(rA, nxt, "vector")
                evac_copy(rB, nxtT, "scalar")
                rC = mm4("R", nxtT, "R", C.pA.next)
                rD = mm4("M4", "R", nxtT, nextX)
                evac_add(rC, "R")
                evac_add(rD, "M4")
                cur, curT, nxt, nxtT = nxt, nxtT, cur, curT
            for msk, mkey in ((d32, "d32"), (d64, "d64"), (d128, "d128")):
                masked("P2", "N", msk, mkey)
                masked("PT2", "NT", msk, mkey)
                rY = mm4("M2", "PT2", "R", C.pA.next)
                rYt = mm4("M3", "P2", "M4", nextX)
                evac_copy(rY, "M2", "vector")
                evac_copy(rYt, "M3", "scalar")
                rZ = mm4("R", "M4", "M2", C.pA.next)
                rZt = mm4("M4", "R", "M3", nextX)
                evac_add(rZ, "R")
                evac_add(rZt, "M4")
            scores(specs[2:5])
            if RW_STOP == 3:
                P.pop(); P.pop()
                return
            for n in range(n0, n0 + nb):
                tsl = slice(n * 128, (n + 1) * 128)
                ptb, ptbk = PTb.next()
                utb, utbk = UTb.next()
                yt, ytk = ytr.next()
                for hh in range(2):
                    p0 = hh * 64
                    u = uidx(n, hh)
                    g = u // 4
                    psP, pkP = nextX() if hh == 0 else C.pA.next()
                    P.op("tensor", lambda e, psP=psP, p0=p0, tsl=tsl: e.matmul(out=psP[:, 0:64], lhsT=aT[p0:p0 + 64, tsl], rhs=STb[p0:p0 + 64, p0:p0 + 64],
                                                                            start=True, stop=False), reads=[fm("aT", n), "STb"], writes=[pkP])
                    P.op("tensor", lambda e, psP=psP, p0=p0, u=u, n=n: e.matmul(out=psP[:, 0:64], lhsT=Mst["M2"][:, u, :], rhs=vtok[:, n, p0:p0 + 64],
                                                                             start=False, stop=True), reads=[("MM2", g), ("vtok", n)], writes=[pkP])
                    C.evac(ptb[:, p0:p0 + 64], psP[:, 0:64], [pkP], [(ptbk, hh)])
                for hh in range(2):
                    p0 = hh * 64
                    u = uidx(n, hh)
                    g = u // 4
                    psU, pkU = nextX() if hh == 0 else C.pA.next()
                    P.op("tensor", lambda e, psU=psU, p0=p0, u=u, ptb=ptb: e.matmul(out=psU[:, 0:64], lhsT=Mst["R"][:, u, :], rhs=ptb[:, p0:p0 + 64],
                                                                                 start=True, stop=True), reads=[("MR", g), (ptbk, hh)], writes=[pkU])
                    C.evac(utb[:, p0:p0 + 64], psU[:, 0:64], [pkU], [(utbk, hh)])
                for hh in range(2):
                    p0 = hh * 64
                    u = uidx(n, hh)
                    g = u // 4
                    psY, pkY = nextX() if hh == 0 else C.pA.next()
                    P.op("tensor", lambda e, psY=psY, p0=p0, tsl=tsl: e.matmul(out=psY[:, 0:64], lhsT=rT[p0:p0 + 64, tsl], rhs=STb[p0:p0 + 64, p0:p0 + 64],
                                                                            start=True, stop=False), reads=[fm("rT", n), "STb"], writes=[pkY])
                    P.op("tensor", lambda e, psY=psY, p0=p0, u=u, utb=utb: e.matmul(out=psY[:, 0:64], lhsT=Mst["M3"][:, u, :], rhs=utb[:, p0:p0 + 64],
                                                                                 start=False, stop=False), reads=[("MM3", g), (utbk, hh)], writes=[pkY])
                    P.op("tensor", lambda e, psY=psY, p0=p0, u=u, n=n: e.matmul(out=psY[:, 0:64], lhsT=Mst["M4"][:, u, :], rhs=vtok[:, n, p0:p0 + 64],
                                                                             start=False, stop=True), reads=[("MM4", g), ("vtok", n)], writes=[pkY])
                    C.evac(yt[:, p0:p0 + 64], psY[:, 0:64], [pkY], [(ytk, hh)])
                tile = tile0 + n
                P.dma("sync", ysc[tile * 128:(tile + 1) * 128, c * 128:(c + 1) * 128], yt[:], reads=[(ytk, 0), (ytk, 1)], writes=[("ysc", tile, c)], semkey=ytk)
                psS, pkS = C.pA.next()
                P.op("tensor", lambda e, psS=psS, n=n, utb=utb: e.matmul(out=psS[:, 0:128], lhsT=btok[:, n, :], rhs=utb[:], start=True, stop=False),
                     reads=[("btok", n), (utbk, 0), (utbk, 1)], writes=[pkS])
                P.op("tensor", lambda e, psS=psS, n=n: e.matmul(out=psS[:, 0:128], lhsT=ktok[:, n, :], rhs=vtok[:, n, :], start=False, stop=True),
                     reads=[("ktok", n), ("vtok", n)], writes=[pkS])
                P.op("vector", lambda e, psS=psS: e.tensor_tensor(out=STf[:], in0=STf[:], in1=psS[:, 0:128], op=ALU.add), reads=[pkS, "STf"], writes=["STf"])
                P.op("vector", lambda e, n=n: e.tensor_scalar(out=STf[:], in0=STf[:], scalar1=gam[:, n:n + 1], scalar2=None, op0=ALU.mult),
                     reads=["STf", fm("gam", n)], writes=["STf"])
                P.op("scalar", lambda e: e.copy(out=STb[:], in_=STf[:]), reads=["STf"], writes=["STb"])
        if RW_STOP == 4:
            P.pop(); P.pop()
            return
    P.pop()
    P.push()
    NH, DH = 16, 64
    gu1 = P.sb([128, D], BF16, "r_gu1")
    gu2 = P.sb([32, D], BF16, "r_gu2")
    wo = P.sb([128, 8, D], BF16, "r_wo")
    P.dma("gpsimd", gu1[:], W["rw_g_up"][li][0:128, :], writes=["gu1"], semkey="r_gu1")
    P.dma("gpsimd", gu2[:], W["rw_g_up"][li][128:160, :], writes=["gu2"], semkey="r_gu2")
    wo_view = W["rw_w_out"][li].rearrange("(c p) n -> p c n", p=128)
    P.dma("gpsimd", wo[:, :, 0:512], wo_view[:, :, 0:512], writes=["wo"], semkey="r_wo")
    P.dma("gpsimd", wo[:, :, 512:1024], wo_view[:, :, 512:1024], writes=["wo"], semkey="r_wo")
    lng = P.sb([128, D], F32, "r_lng")
    lnb = P.sb([128, D], F32, "r_lnb")
    P.dma("sync", lng[:], W["rw_ln_g"][li:li + 1, :].broadcast_to([128, D]), writes=["lng"], semkey="r_lng")
    P.dma("sync", lnb[:], W["rw_ln_b"][li:li + 1, :].broadcast_to([128, D]), writes=["lnb"], semkey="r_lnb")
    ytl = Rot([(P.sb([128, NH, DH], F32, f"r_yl{i}"), f"r_yl{i}") for i in range(2)])
    vtl = Rot([(P.sb([128, NH, DH], BF16, f"r_vl{i}"), f"r_vl{i}") for i in range(2)])
    sqt = P.sb([128, NH, DH], F32, "r_sqt")
    st = Rot([(P.sb([128, 4, NH], F32, f"r_st{i}"), f"r_st{i}") for i in range(2)])
    gtl = Rot([(P.sb([128, D], F32, f"r_gt{i}"), f"r_gt{i}") for i in range(2)])
    ogr = Rot([(P.sb([128, D], BF16, f"r_og{i}"), f"r_og{i}") for i in range(2)])
    ogTr = Rot([(P.sb([128, 8, 128], BF16, f"r_ogT{i}"), f"r_ogT{i}") for i in range(2)])
    for n in range(NT):
        tile = tile0 + n
        tsl = slice(n * 128, (n + 1) * 128)
        yl, ylk = ytl.next()
        vl, vlk = vtl.next()
        P.dma("sync", yl[:].rearrange("p h d -> p (h d)"), ysc[tile * 128:(tile + 1) * 128, :], reads=[("ysc", tile, c) for c in range(8)], writes=[ylk], semkey=ylk)
        P.dma("sync", vl[:].rearrange("p h d -> p (h d)"), vsc[tile * 128:(tile + 1) * 128, :], reads=[("vsc", c) for c in range(8)], writes=[vlk], semkey=vlk)
        gt, gtk = gtl.next()
        for hf in range(2):
            ps, pk = C.pA.next()
            P.op("tensor", lambda e, ps=ps, hf=hf, tsl=tsl: e.matmul(out=ps[:], lhsT=tg1[:, tsl], rhs=gu1[:, hf * 512:(hf + 1) * 512], start=True, stop=False),
                 reads=lkeys("tg1") + ["gu1"], writes=[pk])
            P.op("tensor", lambda e, ps=ps, hf=hf, tsl=tsl: e.matmul(out=ps[:], lhsT=tg2[:, tsl], rhs=gu2[:, hf * 512:(hf + 1) * 512], start=False, stop=True),
                 reads=lkeys("tg2") + ["gu2"], writes=[pk])
            C.evac(gt[:, hf * 512:(hf + 1) * 512], ps[:], [pk], [(gtk, hf)])
        s4, sk4 = st.next()
        P.op("vector", lambda e, s4=s4, yl=yl: e.tensor_reduce(out=s4[:, 0, :], in_=yl[:], axis=AX.X, op=ALU.add), reads=[ylk], writes=[sk4])
        P.op("scalar", lambda e, yl=yl: e.activation(out=sqt[:], in_=yl[:], func=AF.Square), reads=[ylk], writes=["sqt"])
        P.op("vector", lambda e, s4=s4: e.tensor_reduce(out=s4[:, 1, :], in_=sqt[:], axis=AX.X, op=ALU.add), reads=["sqt", sk4], writes=[sk4])
        P.op("vector", lambda e, s4=s4: e.tensor_scalar(out=s4[:, 0:2, :], in0=s4[:, 0:2, :], scalar1=1.0 / DH, scalar2=None, op0=ALU.mult), reads=[sk4], writes=[sk4])
        P.op("vector", lambda e, s4=s4: e.tensor_tensor(out=s4[:, 2, :], in0=s4[:, 0, :], in1=s4[:, 0, :], op=ALU.mult), reads=[sk4], writes=[sk4])
        P.op("vector", lambda e, s4=s4: e.tensor_tensor(out=s4[:, 2, :], in0=s4[:, 1, :], in1=s4[:, 2, :], op=ALU.subtract), reads=[sk4], writes=[sk4])
        P.op("scalar", lambda e, s4=s4: e.activation(out=s4[:, 2, :], in_=s4[:, 2, :], func=AF.Sqrt, bias=64e-5), reads=[sk4], writes=[sk4])
        P.op("vector", lambda e, s4=s4: e.reciprocal(out=s4[:, 3, :], in_=s4[:, 2, :]), reads=[sk4], writes=[sk4])
        P.op("vector", lambda e, s4=s4, yl=yl: e.tensor_tensor(out=yl[:], in0=yl[:], in1=s4[:, 0, :].unsqueeze(2).broadcast_to([128, NH, DH]), op=ALU.subtract),
             reads=[ylk, sk4], writes=[ylk])
        P.op("vector", lambda e, s4=s4, yl=yl: e.tensor_tensor(out=yl[:], in0=yl[:], in1=s4[:, 3, :].unsqueeze(2).broadcast_to([128, NH, DH]), op=ALU.mult),
             reads=[ylk, sk4], writes=[ylk])
        ylf = yl[:].rearrange("p h d -> p (h d)")
        P.op("vector", lambda e, ylf=ylf: e.tensor_tensor(out=ylf, in0=ylf, in1=lng[:], op=ALU.mult), reads=[ylk, "lng"], writes=[ylk])
        P.op("vector", lambda e, ylf=ylf: e.tensor_tensor(out=ylf, in0=ylf, in1=lnb[:], op=ALU.add), reads=[ylk, "lnb"], writes=[ylk])
        P.op("vector", lambda e, n=n, vl=vl: e.tensor_tensor(out=sqt[:], in0=vl[:], in1=bsc[:, n, :].unsqueeze(2).broadcast_to([128, NH, DH]), op=ALU.mult),
             reads=[vlk, ("bsc", n), "sqt", sk4], writes=["sqt"])
        P.op("vector", lambda e, yl=yl: e.tensor_tensor(out=yl[:], in0=yl[:], in1=sqt[:], op=ALU.add), reads=[ylk, "sqt"], writes=[ylk])
        og, ogk = ogr.next()
        P.op("vector", lambda e, og=og, ylf=ylf, gt=gt: e.tensor_tensor(out=og[:], in0=ylf, in1=gt[:], op=ALU.mult), reads=[ylk, (gtk, 0), (gtk, 1)], writes=[ogk])
        pT, ptk = C.pT.next()
        for c in range(8):
            P.op("tensor", lambda e, c=c, pT=pT, og=og: e.transpose(out=pT[:, c, :], in_=og[:, c * 128:(c + 1) * 128], identity=C.ident[:]),
                 reads=[ogk, "ident"], writes=[ptk])
        ogT, ogTk = ogTr.next()
        C.evac(ogT[:], pT[:], [ptk], [ogTk])
        ht, hk = C.ht.next()
        P.dma("sync", ht[:], hsrc[tile * 128:(tile + 1) * 128, :], reads=hkeys(tile), writes=[hk], semkey=hk)
        for hf in range(2):
            ps, pk = C.pA.next()
            for c in range(8):
                P.op("tensor", lambda e, c=c, ps=ps, ogT=ogT, hf=hf: e.matmul(out=ps[:], lhsT=ogT[:, c, :], rhs=wo[:, c, hf * 512:(hf + 1) * 512],
                                                                          start=(c == 0), stop=(c == 7)), reads=["wo", ogTk], writes=[pk])
            P.op("vector", lambda e, ps=ps, ht=ht, hf=hf: e.tensor_tensor(out=ht[:, hf * 512:(hf + 1) * 512], in0=ht[:, hf * 512:(hf + 1) * 512], in1=ps[:], op=ALU.add),
                 reads=[pk, hk], writes=[hk])
        P.dma("sync", hdst[tile * 128:(tile + 1) * 128, :], ht[:], reads=[hk], writes=hkeys(tile), semkey=hk)
    P.pop()
    P.pop()


WNAMES = dict(
    mlp_norm_g=[4, D], mlp_w_up=[4, D, DFF], mlp_w_down=[4, DFF, D], final_norm_g=[1, D],
    fox_norm_g=[1, D], fox_w_in=[1, D, 4112], fox_b_f=[1, 16], fox_w_out=[1, D, D],
    ml_norm_g=[1, D], ml_w_in=[1, D, 3088], ml_conv_w=[1, 4, 1024], ml_conv_b=[1, 1024], ml_b_i=[1, 8], ml_b_f=[1, 8],
    ml_head_g=[1, 1024], ml_w_out=[1, D, D],
    s5_norm_g=[1, D], s5_a_re=[1, 64, 64], s5_a_im=[1, 64, 64], s5_log_dt=[1, 64], s5_b_re=[1, 64, 64, 16], s5_b_im=[1, 64, 64, 16],
    rw_norm_g=[1, D], rw_mu=[1, 6, D], rw_w_in=[1, D, 3360], rw_w0=[1, D], rw_w_up=[1, 64, D], rw_a0=[1, D], rw_a_up=[1, 64, D],
    rw_g_up=[1, 160, D], rw_k_k=[1, D], rw_k_a=[1, D], rw_r_k=[1, D], rw_ln_g=[1, D], rw_ln_b=[1, D], rw_w_out=[1, D, D],
    s5_c_re=[1, 64, 16, 64], s5_c_im=[1, 64, 16, 64], s5_d=[1, D], s5_w_glu=[1, D, 2 * D], s5_b_glu=[1, 2 * D],
)


def build(nseq, L, layers=("s5", "ml", "fox", "rw"), mlp=True):
    nc = bass.Bass("TRN2", target_bir_lowering=False)
    T = nseq * L
    NT = T // 128
    NTS = L // 128

    def din(name, shape):
        return nc.dram_tensor(name, list(shape), F32, kind="ExternalInput").ap()

    x = din("x", [T, D])
    y = nc.dram_tensor("y", [T, D], F32, kind="ExternalOutput").ap()
    hb = nc.dram_tensor("hb", [T, D], F32, kind="Internal").ap()
    osc = nc.dram_tensor("osc", [T, D], BF16, kind="Internal").ap()
    TBK = min(L, 1024)
    tabs = nc.dram_tensor("tabs", [32, L // TBK, 128, 2, TBK], F32, kind="Internal").ap()
    ysc = nc.dram_tensor("ysc", [T, D], F32, kind="Internal").ap()
    vsc = nc.dram_tensor("vsc", [T, D], BF16, kind="Internal").ap()
    W = {k: din(k, s) for k, s in WNAMES.items()}
    P = Prog(nc)
    C = Ctx(nc, P)
    src = x
    for li, kind in enumerate(layers):
        for s in range(nseq):
            if kind == "fox":
                stage_fox(C, src, hb, s * NTS, L, W, 0, osc)
            elif kind == "ml":
                stage_ml(C, src, hb, s * NTS, L, W, 0, osc)
            elif kind == "s5":
                stage_s5(C, src, hb, s * NTS, L, W, 0, tabs, s == 0)
            elif kind == "rw":
                stage_rw(C, src, hb, s * NTS, L, W, 0, ysc, vsc)
        if kind in ("fox", "ml", "s5", "rw"):
            src = hb
        if mlp:
            for blk in range(T // 1024):
                stage_mlp(C, src, hb, blk * 8, W["mlp_w_up"][li], W["mlp_w_down"][li], W["mlp_norm_g"][li:li + 1, :])
            src = hb
    stage_final(C, src, y, NT, W["final_norm_g"])
    P.finish([("y", n) for n in range(NT)])
    P.emit()
    print("prog stats", P.stats)
    P.close()
    return nc


def make_in_maps(inputs, ncores):
    x = np.ascontiguousarray(inputs["x"], dtype=np.float32)
    B, L, _ = x.shape
    nseq = B // ncores
    shared = {}
    for k, s in WNAMES.items():
        shared[k] = np.ascontiguousarray(np.asarray(inputs[k], dtype=np.float32).reshape(s))
    in_maps = []
    for c in range(ncores):
        m = dict(shared)
        m["x"] = x[c * nseq:(c + 1) * nseq].reshape(nseq * L, D)
        in_maps.append(m)
    return in_maps, nseq, L


def kernel(**inputs):
    ncores = 8
    in_maps, nseq, L = make_in_maps(inputs, ncores)
    nc = build(nseq, L)
    res = run_bass_kernel_spmd(nc, in_maps, core_ids=list(range(ncores)))
    out = np.stack([np.asarray(r["y"]).reshape(nseq, L, D) for r in res.results], axis=0)
    return out.reshape(ncores * nseq, L, D).astype(np.float32)
```
